# Optimizing a Trainium2 kernel written in Bass

```python
import jax, jax.numpy as jnp
from jax import lax
import numpy as np

D_MODEL = 1024
BATCH = 2
SEQ = 16384
DEPTH = 2

N_A_LAYERS = DEPTH // 2
N_B_LAYERS = DEPTH - N_A_LAYERS
N_DENSE = (DEPTH + 1) // 2
N_MOE = DEPTH // 2
POOL_WINDOWS = (2, 4, 8, 16)
N_POOL_GROUPS = 4
POOL_GROUP_DIM = D_MODEL // N_POOL_GROUPS
HEAD_DIM = 128
N_HEADS = D_MODEL // HEAD_DIM
MOBA_BLOCK = 256
MOBA_TOPK = 3
Q_CHUNK = 128
ROPE_THETA = 500000.0
ROPE_DIM = HEAD_DIM // 4
D_FF = 2816
N_EXPERTS = 8
TOP_K_EXPERTS = 2
EXPERT_FF = 3584
PLE_DIM = 256
RMS_EPS = 1e-6
NEG_INF = -1e30

kernel_name = "yoco_pool_moba_moe_trunk"


def rms_norm(x, g):
    xf = x.astype(jnp.float32)
    var = jnp.mean(xf * xf, axis=-1, keepdims=True)
    return (xf * lax.rsqrt(var + RMS_EPS) * g.astype(jnp.float32)).astype(x.dtype)


def partial_rope(x, pos):
    half = ROPE_DIM // 2
    inv_freq = jnp.float32(ROPE_THETA) ** (-(jnp.arange(0, ROPE_DIM, 2, dtype=jnp.float32) / ROPE_DIM))
    ang = pos.astype(jnp.float32)[:, None] * inv_freq[None, :]
    cos, sin = jnp.cos(ang), jnp.sin(ang)
    xf = x.astype(jnp.float32)
    x1, x2, rest = xf[..., :half], xf[..., half:ROPE_DIM], xf[..., ROPE_DIM:]
    out = jnp.concatenate([x1 * cos - x2 * sin, x2 * cos + x1 * sin, rest], axis=-1)
    return out.astype(x.dtype)


def pool_mixer(xn, pool_w, pool_scale):
    B, S, D = xn.shape
    xf = xn.astype(jnp.float32)
    cs = jnp.cumsum(xf, axis=1)
    t = jnp.arange(S)
    outs = []
    for g, w in enumerate(POOL_WINDOWS):
        cg = cs[..., g * POOL_GROUP_DIM:(g + 1) * POOL_GROUP_DIM]
        shifted = jnp.concatenate(
            [jnp.zeros((B, w, POOL_GROUP_DIM), jnp.float32), cg[:, :S - w]], axis=1)
        cnt = jnp.minimum(t + 1, w).astype(jnp.float32)[None, :, None]
        outs.append((cg - shifted) / cnt)
    pooled = jnp.concatenate(outs, axis=-1) - xf
    pooled = pooled.reshape(B, S, N_POOL_GROUPS, POOL_GROUP_DIM).astype(xn.dtype)
    mixed = jnp.einsum('bsgc,gcd->bsgd', pooled, pool_w).reshape(B, S, D)
    return mixed * pool_scale


def shared_kv(h, kv_norm, w_k, w_v):
    B, S, D = h.shape
    kn = rms_norm(h, kv_norm)
    pos = jnp.arange(S)
    k = (kn @ w_k).reshape(B, S, N_HEADS, HEAD_DIM).transpose(0, 2, 1, 3)
    v = (kn @ w_v).reshape(B, S, N_HEADS, HEAD_DIM).transpose(0, 2, 1, 3)
    k = partial_rope(k, pos)
    nb = -(-S // MOBA_BLOCK)
    pad = nb * MOBA_BLOCK - S
    k = jnp.pad(k, ((0, 0), (0, 0), (0, pad), (0, 0)))
    v = jnp.pad(v, ((0, 0), (0, 0), (0, pad), (0, 0)))
    k_blocks = k.reshape(B, N_HEADS, nb, MOBA_BLOCK, HEAD_DIM)
    v_blocks = v.reshape(B, N_HEADS, nb, MOBA_BLOCK, HEAD_DIM)
    k_mean = jnp.mean(k_blocks.astype(jnp.float32), axis=3)
    return k_blocks, v_blocks, k_mean


def moba_attention(q, k_blocks, v_blocks, k_mean):
    B, H, S, hd = q.shape
    nb = k_blocks.shape[2]
    nc = S // Q_CHUNK
    n_sel = min(MOBA_TOPK, nb)
    scale = hd ** -0.5
    q_chunks = q.reshape(B, H, nc, Q_CHUNK, hd).transpose(2, 0, 1, 3, 4)
    bi = jnp.arange(B)[:, None, None]
    hi = jnp.arange(H)[None, :, None]
    blk_ids = jnp.arange(nb)

    def one_chunk(args):
        c, qc = args
        q0 = c * Q_CHUNK
        j = q0 // MOBA_BLOCK
        qf = qc.astype(jnp.float32)
        gate = jnp.einsum('bhqd,bhnd->bhqn', qf, k_mean)
        gate = jnp.where((blk_ids < j)[None, None, None, :], gate, NEG_INF)
        _, sel = lax.top_k(gate, n_sel)
        valid = sel < j
        idx = sel.reshape(B, H, Q_CHUNK * n_sel)
        k_sel = k_blocks[bi, hi, idx].reshape(B, H, Q_CHUNK, n_sel, MOBA_BLOCK, hd)
        v_sel = v_blocks[bi, hi, idx].reshape(B, H, Q_CHUNK, n_sel, MOBA_BLOCK, hd)
        s_sel = jnp.einsum('bhqd,bhqnld->bhqnl', qf, k_sel.astype(jnp.float32)) * scale
        s_sel = jnp.where(valid[..., None], s_sel, NEG_INF)
        s_sel = s_sel.reshape(B, H, Q_CHUNK, n_sel * MOBA_BLOCK)
        k_cur = lax.dynamic_index_in_dim(k_blocks, j, axis=2, keepdims=False)
        v_cur = lax.dynamic_index_in_dim(v_blocks, j, axis=2, keepdims=False)
        s_cur = jnp.einsum('bhqd,bhld->bhql', qf, k_cur.astype(jnp.float32)) * scale
        qpos = q0 + jnp.arange(Q_CHUNK)
        kpos = j * MOBA_BLOCK + jnp.arange(MOBA_BLOCK)
        s_cur = jnp.where((kpos[None, :] <= qpos[:, None])[None, None], s_cur, NEG_INF)
        probs = jax.nn.softmax(jnp.concatenate([s_sel, s_cur], axis=-1), axis=-1)
        p_sel = probs[..., :n_sel * MOBA_BLOCK].reshape(B, H, Q_CHUNK, n_sel, MOBA_BLOCK)
        p_cur = probs[..., n_sel * MOBA_BLOCK:]
        o = (jnp.einsum('bhqnl,bhqnld->bhqd', p_sel, v_sel.astype(jnp.float32))
             + jnp.einsum('bhql,bhld->bhqd', p_cur, v_cur.astype(jnp.float32)))
        return o.astype(q.dtype)

    out = lax.map(one_chunk, (jnp.arange(nc), q_chunks))
    return out.transpose(1, 0, 3, 2, 4).reshape(B, S, H * hd)


def moba_layer(xn, w_q, w_o, k_blocks, v_blocks, k_mean):
    B, S, D = xn.shape
    q = (xn @ w_q).reshape(B, S, N_HEADS, HEAD_DIM).transpose(0, 2, 1, 3)
    q = partial_rope(q, jnp.arange(S))
    return moba_attention(q, k_blocks, v_blocks, k_mean) @ w_o


def swiglu(xn, w1, w3, w2):
    return (jax.nn.silu(xn @ w1) * (xn @ w3)) @ w2


def moe_swiglu(xn, router, w1, w3, w2):
    B, S, D = xn.shape
    t = xn.reshape(B * S, D)
    logits = (t @ router).astype(jnp.float32)
    top_val, top_idx = lax.top_k(logits, TOP_K_EXPERTS)
    top_w = jax.nn.softmax(top_val, axis=-1)
    gates = jnp.sum(jax.nn.one_hot(top_idx, N_EXPERTS, dtype=jnp.float32) * top_w[..., None], axis=1)
    y = jnp.zeros((B * S, D), jnp.float32)
    for e in range(N_EXPERTS):
        y = y + gates[:, e:e + 1] * swiglu(t, w1[e], w3[e], w2[e]).astype(jnp.float32)
    return y.reshape(B, S, D).astype(xn.dtype)


def per_layer_embedding(hn, p_i, gate_w, proj_w):
    return jax.nn.sigmoid(hn @ gate_w) * (p_i @ proj_w)


def setup_inputs(seed: int = 0) -> dict:
    key = jax.random.key(seed)
    ks = jax.random.split(key, 24)
    D = D_MODEL
    f32 = jnp.float32

    def nrm(k, shape, fan_in):
        return jax.random.normal(k, shape, f32) * fan_in ** -0.5

    def gain(k, shape):
        return 1.0 + 0.05 * jax.random.normal(k, shape, f32)

    return {
        "x": jax.random.normal(ks[0], (BATCH, SEQ, D), f32),
        "p": jax.random.normal(ks[1], (DEPTH, BATCH, SEQ, PLE_DIM), f32),
        "pool_norm": gain(ks[2], (N_A_LAYERS, D)),
        "pool_w": nrm(ks[3], (N_A_LAYERS, N_POOL_GROUPS, POOL_GROUP_DIM, POOL_GROUP_DIM), POOL_GROUP_DIM),
        "pool_scale": 0.5 + 0.05 * jax.random.normal(ks[4], (N_A_LAYERS, D), f32),
        "kv_norm": gain(ks[5], (D,)),
        "w_k": nrm(ks[6], (D, D), D),
        "w_v": nrm(ks[7], (D, D), D),
        "attn_norm": gain(ks[8], (N_B_LAYERS, D)),
        "w_q": nrm(ks[9], (N_B_LAYERS, D, D), D),
        "w_o": nrm(ks[10], (N_B_LAYERS, D, D), D),
        "ffn_norm": gain(ks[11], (DEPTH, D)),
        "ffn_w1": nrm(ks[12], (N_DENSE, D, D_FF), D),
        "ffn_w3": nrm(ks[13], (N_DENSE, D, D_FF), D),
        "ffn_w2": nrm(ks[14], (N_DENSE, D_FF, D), D_FF),
        "router": nrm(ks[15], (N_MOE, D, N_EXPERTS), D),
        "exp_w1": nrm(ks[16], (N_MOE, N_EXPERTS, D, EXPERT_FF), D),
        "exp_w3": nrm(ks[17], (N_MOE, N_EXPERTS, D, EXPERT_FF), D),
        "exp_w2": nrm(ks[18], (N_MOE, N_EXPERTS, EXPERT_FF, D), EXPERT_FF),
        "ple_norm": gain(ks[19], (DEPTH, D)),
        "ple_gate": nrm(ks[20], (DEPTH, D, D), D),
        "ple_proj": nrm(ks[21], (DEPTH, PLE_DIM, D), PLE_DIM),
        "final_norm": gain(ks[22], (D,)),
    }


def reference(x, p, pool_norm, pool_w, pool_scale, kv_norm, w_k, w_v, attn_norm, w_q, w_o,
              ffn_norm, ffn_w1, ffn_w3, ffn_w2, router, exp_w1, exp_w3, exp_w2,
              ple_norm, ple_gate, ple_proj, final_norm):
    h = x
    shared = None
    for i in range(DEPTH):
        if i < N_A_LAYERS:
            h = h + pool_mixer(rms_norm(h, pool_norm[i]), pool_w[i], pool_scale[i])
        else:
            b = i - N_A_LAYERS
            if shared is None:
                shared = shared_kv(h, kv_norm, w_k, w_v)
            k_blocks, v_blocks, k_mean = shared
            h = h + moba_layer(rms_norm(h, attn_norm[b]), w_q[b], w_o[b], k_blocks, v_blocks, k_mean)
        hn = rms_norm(h, ffn_norm[i])
        if i % 2 == 0:
            d = i // 2
            h = h + swiglu(hn, ffn_w1[d], ffn_w3[d], ffn_w2[d])
        else:
            m = i // 2
            h = h + moe_swiglu(hn, router[m], exp_w1[m], exp_w3[m], exp_w2[m])
        h = h + per_layer_embedding(rms_norm(h, ple_norm[i]), p[i], ple_gate[i], ple_proj[i])
    return rms_norm(h, final_norm)
```

```python
import numpy as np
from contextlib import ExitStack
import concourse.bass as bass
import concourse.mybir as mybir
from concourse.bass_utils import run_bass_kernel_spmd

F32 = mybir.dt.float32
BF16 = mybir.dt.bfloat16
AF = mybir.ActivationFunctionType
ALU = mybir.AluOpType
AX = mybir.AxisListType

D = 1024
KC = 8
S = 16384
B = 2
G = 512
NGRP = 8
TC = G * NGRP
HALO = 16
GP = 4
NPASS = NGRP // GP
TP = GP * G
D_FF = 2816
E_FF = 3584
NEXP = 8
PLE = 256
EPS = 1e-6
NB = S // 256
ENGS = ("pe", "act", "dve", "pool", "sp")


class _Op:
    __slots__ = ("eng", "fn", "reads", "writes", "dma", "semname", "deps", "signal", "sem", "cnt", "inc")


class _Rec:
    def __init__(self):
        self.call = None

    def __getattr__(self, name):
        def f(*a, **kw):
            self.call = (name, a, kw)
            return self
        return f


class Prog:
    def __init__(self, nc):
        self.nc = nc
        self.ops = []
        self.last_w = {}
        self.readers = {}
        self.eng_sem = {e: nc.alloc_semaphore(name="s_" + e) for e in ENGS}
        self.eng_cnt = {e: 0 for e in ENGS}
        self.dma_sem = {}
        self.dma_cnt = {}
        self.waited = {e: {} for e in ENGS}

    def add(self, eng, fn, reads=(), writes=(), dma=False, semname=None):
        op = _Op()
        rec = _Rec()
        fn(rec)
        op.eng, op.fn, op.reads, op.writes, op.dma = eng, rec.call, tuple(reads), tuple(writes), dma
        op.semname = semname
        op.deps, op.signal, op.sem, op.cnt, op.inc = [], dma, None, 0, 16
        self.ops.append(op)
        return op

    def cc(self, ins_ap, outs_ap, groups, reads, writes, semname):
        op = self.add("pool", lambda e: e.collective_compute("AllGather", ALU.bypass, replica_groups=groups, ins=[ins_ap], outs=[outs_ap]),
                      reads, writes, True, semname)
        op.inc = 1
        return op

    def dma(self, eng, out, in_, reads=(), writes=(), semname=None):
        if semname is None:
            k = writes[0] if writes else reads[0]
            semname = "d_" + "_".join(str(x) for x in k)
        return self.add(eng, lambda e: e.dma_start(out=out, in_=in_), reads, writes, True, semname)

    def flush(self, barrier=True):
        nc = self.nc
        ops, self.ops = self.ops, []
        for op in ops:
            deps = {}
            for k in op.reads:
                w = self.last_w.get(k)
                if w is not None:
                    deps[id(w)] = w
            for k in op.writes:
                w = self.last_w.get(k)
                if w is not None:
                    deps[id(w)] = w
                for r in self.readers.get(k, ()):
                    deps[id(r)] = r
            for k in op.reads:
                self.readers.setdefault(k, []).append(op)
            for k in op.writes:
                self.last_w[k] = op
                self.readers[k] = []
            for d in deps.values():
                if d is op:
                    continue
                if d.eng == "pe" and op.eng == "pe" and not d.dma and not op.dma:
                    continue
                op.deps.append(d)
                d.signal = True
        for op in ops:
            if not op.signal:
                continue
            if op.dma:
                if op.semname not in self.dma_sem:
                    self.dma_sem[op.semname] = nc.alloc_semaphore(name=op.semname[:40])
                    self.dma_cnt[op.semname] = 0
                self.dma_cnt[op.semname] += op.inc
                op.sem, op.cnt = op.semname, self.dma_cnt[op.semname]
            else:
                self.eng_cnt[op.eng] += 1
                op.sem, op.cnt = op.eng, self.eng_cnt[op.eng]
        per = {e: [o for o in ops if o.eng == e] for e in ENGS}

        def semh(name):
            return self.eng_sem[name] if name in self.eng_sem else self.dma_sem[name]

        def emit(e, eng):
            wd = self.waited[e]
            for op in per[e]:
                need = {}
                for d in op.deps:
                    if need.get(d.sem, 0) < d.cnt:
                        need[d.sem] = d.cnt
                for s, c in need.items():
                    if wd.get(s, 0) < c:
                        eng.wait_ge(semh(s), c)
                        wd[s] = c
                ins = getattr(eng, op.fn[0])(*op.fn[1], **op.fn[2])
                if op.signal:
                    if op.dma:
                        ins.then_inc(self.dma_sem[op.sem], op.inc)
                    else:
                        ins.then_inc(self.eng_sem[e], 1)
            if e == "sp":
                for s, c in self.dma_cnt.items():
                    if wd.get(s, 0) < c:
                        eng.wait_ge(self.dma_sem[s], c)
                        wd[s] = c

        with nc.Block() as block:
            block.tensor(lambda t: emit("pe", t))
            block.scalar(lambda t: emit("act", t))
            block.vector(lambda t: emit("dve", t))
            block.gpsimd(lambda t: emit("pool", t))
            block.sync(lambda t: emit("sp", t))
        if barrier:
            nc.all_engine_barrier()
            self.last_w = {}
            self.readers = {}


class Ctx:
    def __init__(self, nc, es):
        self.nc = nc
        self.es = es
        self.p = Prog(nc)
        self.pb = [es.enter_context(nc.psum_tensor("pb%d" % i, [128, 512], F32)) for i in range(8)]
        self.rr = {}

    def sb(self, es, name, shape, dt):
        self.uid = getattr(self, "uid", 0) + 1
        return es.enter_context(self.nc.sbuf_tensor("sb%d_%s" % (self.uid, name), shape, dt))

    def rot(self, name, n):
        v = self.rr.get(name, 0)
        self.rr[name] = v + 1
        return v % n


def load_w(cx, dst, dkey, w_ap, c0, c1, kc):
    src = w_ap.rearrange("(k p) n -> p k n", p=128)[:, :, c0:c1]
    cx.p.dma("pool", dst[:, :kc, :c1 - c0], src, writes=[dkey])


VEC_NAMES = ["pool_norm", "pool_scale", "ffn_norm0", "ple_norm0", "kv_norm", "attn_norm", "ffn_norm1", "ple_norm1", "final_norm"]
VI = {n: i for i, n in enumerate(VEC_NAMES)}


def common_consts(cx, es, vecs_d, cst_d):
    nc, p = cx.nc, cx.p
    cx.vecs = cx.sb(es, "vecs", [128, len(VEC_NAMES), KC], F32)
    cx.ones = cx.sb(es, "ones", [128, 128], BF16)
    cx.epsb = cx.sb(es, "epsb", [128, 1], F32)
    cx.rsw = cx.sb(es, "rsw", [32, 32], BF16)
    p.dma("sp", cx.vecs[:, :, :], vecs_d, writes=["vecs"])
    p.dma("pool", cx.rsw[:, :], cst_d, writes=["rsw"])
    p.add("dve", lambda e: e.memset(cx.ones[:, :], 1.0), writes=["ones"])
    p.add("dve", lambda e: e.memset(cx.epsb[:, :], EPS), writes=["ones"])


def ffn_alloc(cx, es, tag):
    SL = 4
    return dict(
        w1s=[cx.sb(es, tag + "w1s%d" % i, [128, KC, SL * 128], BF16) for i in range(2)],
        w3s=[cx.sb(es, tag + "w3s%d" % i, [128, KC, SL * 128], BF16) for i in range(2)],
        w2s=[cx.sb(es, tag + "w2s%d" % i, [128, SL, D], BF16) for i in range(2)],
        sl_t=[cx.sb(es, tag + "silu%d" % i, [128, G], F32) for i in range(2)],
        gbuf=[cx.sb(es, tag + "g%d" % i, [128, SL, G], BF16) for i in range(2)])


def ffn_prefetch(cx, bufs, w1_d, w3_d, w2_d, nff, tag):
    SL = 4
    nf = min(SL, nff // 128)
    ws = cx.rot(tag + "w", 2)
    bufs["pre"] = ws
    load_w(cx, bufs["w1s"][ws], (tag + "w1s", ws), w1_d, 0, nf * 128, KC)
    load_w(cx, bufs["w3s"][ws], (tag + "w3s", ws), w3_d, 0, nf * 128, KC)
    cx.p.dma("pool", bufs["w2s"][ws][:, :nf, :], w2_d[0:nf * 128, :].rearrange("(k p) n -> p k n", p=128), writes=[(tag + "w2s", ws)])


def ffn_stage(cx, bufs, h, hn, w1_d, w3_d, w2_d, nff, ngrp, tag, gate=None):
    p = cx.p
    nfc = nff // 128
    SL = 4
    w1s, w3s, w2s, sl_t, gbuf = bufs["w1s"], bufs["w3s"], bufs["w2s"], bufs["sl_t"], bufs["gbuf"]
    nsl = (nfc + SL - 1) // SL
    units = [(s_, gl) for s_ in range(nsl) for gl in range(ngrp)]
    wslot = {}

    def load(s_):
        f0 = s_ * SL
        nf = min(SL, nfc - f0)
        if s_ == 0 and bufs.get("pre") is not None:
            wslot[0] = bufs.pop("pre")
            return
        ws = cx.rot(tag + "w", 2)
        wslot[s_] = ws
        load_w(cx, w1s[ws], (tag + "w1s", ws), w1_d, f0 * 128, (f0 + nf) * 128, KC)
        load_w(cx, w3s[ws], (tag + "w3s", ws), w3_d, f0 * 128, (f0 + nf) * 128, KC)
        src2 = w2_d[f0 * 128:(f0 + nf) * 128, :].rearrange("(k p) n -> p k n", p=128)
        p.dma("pool", w2s[ws][:, :nf, :], src2, writes=[(tag + "w2s", ws)])

    def emitH(k):
        s_, gl = units[k]
        if gl == 0:
            load(s_)
        ws = wslot[s_]
        nf = min(SL, nfc - s_ * SL)
        t0 = gl * G
        gs = k % 2
        for f in range(nf):
            b1 = cx.rot(tag + "b1", 2)
            b3 = 2 + cx.rot(tag + "b3", 2)
            ss = cx.rot(tag + "sl", 2)
            for c in range(KC):
                p.add("pe", lambda e: e.matmul(cx.pb[b1][:, :], w1s[ws][:, c, f * 128:(f + 1) * 128], hn[:, c, t0:t0 + G],
                                               start=(c == 0), stop=(c == KC - 1)),
                      reads=[(tag + "w1s", ws), ("hn", gl, c)], writes=[("pb", b1)])
            for c in range(KC):
                p.add("pe", lambda e: e.matmul(cx.pb[b3][:, :], w3s[ws][:, c, f * 128:(f + 1) * 128], hn[:, c, t0:t0 + G],
                                               start=(c == 0), stop=(c == KC - 1)),
                      reads=[(tag + "w3s", ws), ("hn", gl, c)], writes=[("pb", b3)])
            p.add("act", lambda e: e.activation(out=sl_t[ss][:, :], in_=cx.pb[b1][:, :], func=AF.Silu),
                  reads=[("pb", b1)], writes=[(tag + "sl", ss)])
            if gate is None:
                p.add("dve", lambda e: e.tensor_tensor(out=gbuf[gs][:, f, :], in0=sl_t[ss][:, :], in1=cx.pb[b3][:, :], op=ALU.mult),
                      reads=[("pb", b3), (tag + "sl", ss)], writes=[(tag + "g", gs, f)])
            else:
                p.add("dve", lambda e: e.tensor_tensor(out=sl_t[ss][:, :], in0=sl_t[ss][:, :], in1=cx.pb[b3][:, :], op=ALU.mult),
                      reads=[("pb", b3), (tag + "sl", ss)], writes=[(tag + "sl", ss)])
                p.add("pool", lambda e: e.tensor_tensor(out=gbuf[gs][:, f, :], in0=sl_t[ss][:, :], in1=gate[0][:, gl, :], op=ALU.mult),
                      reads=[(tag + "sl", ss), gate[1] + (gl,)], writes=[(tag + "g", gs, f)])

    def emitY(k):
        s_, gl = units[k]
        ws = wslot[s_]
        nf = min(SL, nfc - s_ * SL)
        t0 = gl * G
        gs = k % 2
        for oc in range(KC):
            by = (4, 5, 7)[cx.rot(tag + "by", 3)]
            for f in range(nf):
                p.add("pe", lambda e: e.matmul(cx.pb[by][:, :], w2s[ws][:, f, oc * 128:(oc + 1) * 128], gbuf[gs][:, f, :],
                                               start=(f == 0), stop=(f == nf - 1)),
                      reads=[(tag + "w2s", ws), (tag + "g", gs, f)], writes=[("pb", by)])
            p.add("dve", lambda e: e.tensor_tensor(out=h[:, oc, t0:t0 + G], in0=h[:, oc, t0:t0 + G], in1=cx.pb[by][:, :], op=ALU.add),
                  reads=[("pb", by), ("h", gl, oc)], writes=[("h", gl, oc)])

    emitH(0)
    for k in range(len(units)):
        if k + 1 < len(units):
            emitH(k + 1)
        emitY(k)


def ple_alloc(cx, es, ngrp, tag):
    return dict(wg=cx.sb(es, tag + "wg", [128, KC, D], BF16), wp=cx.sb(es, tag + "wp", [128, 2, D], BF16),
                pt=cx.sb(es, tag + "pt", [128, 2, ngrp * G], BF16), sg=[cx.sb(es, tag + "sg%d" % i, [128, G], F32) for i in range(2)])


def ple_load(cx, b, pT_d, tok0, gate_d, proj_d, ngrp, tag):
    load_w(cx, b["wg"], (tag + "wg",), gate_d, 0, D, KC)
    load_w(cx, b["wp"], (tag + "wp",), proj_d, 0, D, 2)
    cx.p.dma("pool", b["pt"][:, :, :], pT_d[:, :, tok0:tok0 + ngrp * G], writes=[(tag + "pt",)])


def ple_stage(cx, b, h, hn, ngrp, tag):
    p = cx.p
    wg, wp, pt, sg = b["wg"], b["wp"], b["pt"], b["sg"]
    for gl in range(ngrp):
        t0 = gl * G
        for oc in range(KC):
            b1 = (0, 1, 4)[cx.rot(tag + "b1", 3)]
            b3 = (2, 3, 5)[cx.rot(tag + "b3", 3)]
            ss = cx.rot(tag + "sg", 2)
            for c in range(KC):
                p.add("pe", lambda e, c=c, oc=oc, b1=b1: e.matmul(cx.pb[b1][:, :], wg[:, c, oc * 128:(oc + 1) * 128], hn[:, c, t0:t0 + G],
                                                                 start=(c == 0), stop=(c == KC - 1)),
                      reads=[(tag + "wg",), ("hn", gl, c)], writes=[("pb", b1)])
            for c in range(2):
                p.add("pe", lambda e, c=c, oc=oc, b3=b3: e.matmul(cx.pb[b3][:, :], wp[:, c, oc * 128:(oc + 1) * 128], pt[:, c, t0:t0 + G],
                                                                 start=(c == 0), stop=(c == 1)),
                      reads=[(tag + "wp",), (tag + "pt",)], writes=[("pb", b3)])
            p.add("act", lambda e, b1=b1, ss=ss: e.activation(out=sg[ss][:, :], in_=cx.pb[b1][:, :], func=AF.Sigmoid),
                  reads=[("pb", b1)], writes=[(tag + "sg", ss)])
            p.add("dve", lambda e, b3=b3, ss=ss: e.tensor_tensor(out=sg[ss][:, :], in0=sg[ss][:, :], in1=cx.pb[b3][:, :], op=ALU.mult),
                  reads=[("pb", b3), (tag + "sg", ss)], writes=[(tag + "sg", ss)])
            p.add("dve", lambda e, oc=oc, ss=ss: e.tensor_tensor(out=h[:, oc, t0:t0 + G], in0=h[:, oc, t0:t0 + G], in1=sg[ss][:, :], op=ALU.add),
                  reads=[(tag + "sg", ss), ("h", gl, oc)], writes=[("h", gl, oc)])


def rmsnorm_view(cx, h, hn, gl, t0, gain, sq, rstd, tag, dst_local=False, dkey=None):
    p = cx.p
    ps = cx.pb[7]
    hkeys = [("h", gl, c) for c in range(KC)]
    for c in range(KC):
        p.add("act", lambda e, c=c: e.activation(out=sq[:, c, :G], in_=h[:, c, t0:t0 + G], func=AF.Square),
              reads=[hkeys[c]], writes=[(tag + "sq", c)])
    for c in range(KC):
        p.add("pe", lambda e, c=c: e.matmul(ps[:, :], cx.ones[:, :], sq[:, c, :G], start=(c == 0), stop=(c == KC - 1)),
              reads=[(tag + "sq", c), "ones"], writes=[("pb", 7)])
    p.add("act", lambda e: e.activation(out=rstd[:, :G], in_=ps[:, :], func=AF.Sqrt, bias=cx.epsb[:, 0:1], scale=1.0 / D),
          reads=[("pb", 7), "ones"], writes=[tag + "rstd"])
    p.add("dve", lambda e: e.reciprocal(out=rstd[:, :G], in_=rstd[:, :G]), reads=[tag + "rstd"], writes=[tag + "rstd"])
    for c in range(KC):
        d0 = 0 if dst_local else t0
        p.add("dve", lambda e, c=c: e.scalar_tensor_tensor(out=hn[:, c, d0:d0 + G], in0=h[:, c, t0:t0 + G], scalar=gain[:, c:c + 1],
                                                          in1=rstd[:, :G], op0=ALU.mult, op1=ALU.mult),
              reads=[hkeys[c], tag + "rstd", "vecs"], writes=[dkey if dkey is not None else ("hn", gl, c)])


POOL_W = (2, 4, 8, 16)


def emit_l1(cx, A, npass=NPASS):
    dbg = False
    p = cx.p
    xT, pT, cntfix_d, ropeC_d, ropeS_d, pool_w_d = A["xT"], A["pT0"], A["cntfix"], A["ropeC"], A["ropeS"], A["pool_w"]
    w1_d, w3_d, w2_d, pg_d, pp_d = A["ffn_w1"], A["ffn_w3"], A["ffn_w2"], A["ple_gate0"], A["ple_proj0"]
    wk_d, wv_d, wq_d = A["w_k"], A["w_v"], A["w_q"]
    hT_o, KT_s, QT_o, V_s, km_o = A["hscr"], A["KTscr"], A["QTscr"], A["Vscr"], A["kmscr"]
    with ExitStack() as es0:
        h = cx.sb(es0, "h", [128, KC, TP], F32)
        hn = cx.sb(es0, "hn", [128, KC, TP], BF16)
        sq = cx.sb(es0, "sq", [128, KC, G + HALO], BF16)
        rstd = cx.sb(es0, "rstd", [128, G + HALO], F32)
        kmT = cx.sb(es0, "kmT", [128, KC, 2 * NGRP], F32)
        vec = lambda n: cx.vecs[:, VI[n], :]
        for pp in range(npass):
            with ExitStack() as es:
                xhs = [cx.sb(es, "xh%d" % i, [128, KC, G + HALO], F32) for i in range(2)]
                xn = cx.sb(es, "xn", [128, KC, G + HALO], F32)
                st = [cx.sb(es, "st%d" % i, [128, G + HALO], F32) for i in range(4)]
                stp = [cx.sb(es, "stp%d" % i, [128, G + HALO], F32) for i in range(4)]
                pooled = cx.sb(es, "pooled", [128, KC, G], BF16)
                pw = cx.sb(es, "pw", [128, 4, 2, 256], BF16)
                cfix = cx.sb(es, "cfix", [128, KC, HALO], F32)
                tmpf = cx.sb(es, "tmpf", [128, HALO], F32)
                NCOL = G + HALO
                for g4 in range(4):
                    p.dma("pool", pw[:, g4, :, :], pool_w_d[g4].rearrange("(k p) n -> p k n", p=128), writes=[("pw", g4)])
                p.dma("sp", cfix[:, :, :], cntfix_d, writes=["cfix"])
                for gl in range(GP):
                    g = pp * GP + gl
                    t0 = gl * G
                    xh = xhs[gl % 2]
                    xk = ("xh", gl % 2)
                    p.dma("sp", xh[:, :, :], xT[:, :, g, :], writes=[xk])
                    for c in range(KC):
                        p.add("act", lambda e, c=c: e.activation(out=sq[:, c, :], in_=xh[:, c, :], func=AF.Square),
                              reads=[xk], writes=[("psq", c)])
                    for (b, lo, hi) in ((6, 0, HALO), (7, HALO, NCOL)):
                        for c in range(KC):
                            p.add("pe", lambda e, c=c, b=b, lo=lo, hi=hi: e.matmul(cx.pb[b][:, :hi - lo], cx.ones[:, :], sq[:, c, lo:hi],
                                                                                  start=(c == 0), stop=(c == KC - 1)),
                                  reads=[("psq", c), "ones"], writes=[("pb", b)])
                        p.add("act", lambda e, b=b, lo=lo, hi=hi: e.activation(out=rstd[:, lo:hi], in_=cx.pb[b][:, :hi - lo], func=AF.Sqrt,
                                                                              bias=cx.epsb[:, 0:1], scale=1.0 / D),
                              reads=[("pb", b), "ones"], writes=["prstd"])
                    p.add("dve", lambda e: e.reciprocal(out=rstd[:, :], in_=rstd[:, :]), reads=["prstd"], writes=["prstd"])
                    gain = vec("pool_norm")
                    for c in range(KC):
                        p.add("dve", lambda e, c=c: e.scalar_tensor_tensor(out=xn[:, c, :], in0=xh[:, c, :], scalar=gain[:, c:c + 1],
                                                                          in1=rstd[:, :], op0=ALU.mult, op1=ALU.mult),
                              reads=[xk, "prstd", "vecs"], writes=[("xn", c)])
                    for c in range(KC):
                        w = POOL_W[c // 2]
                        cur = xn[:, c, :]
                        curk = ("xn", c)
                        sh = 1
                        lo = 0
                        i = 0
                        en = "pool" if c in (4, 6) else "dve"
                        stt = stp if en == "pool" else st
                        while sh < w:
                            lo2 = lo + sh
                            p.add(en, lambda e, cur=cur, lo2=lo2, sh=sh, i=i: e.tensor_tensor(out=stt[i][:, lo2:NCOL], in0=cur[:, lo2:NCOL],
                                                                                             in1=cur[:, lo2 - sh:NCOL - sh], op=ALU.add),
                                  reads=[curk], writes=[("st", en, i)])
                            cur = stt[i][:, :]
                            curk = ("st", en, i)
                            lo = lo2
                            sh *= 2
                            i += 1
                        p.add("dve", lambda e, c=c, cur=cur, w=w: e.scalar_tensor_tensor(out=pooled[:, c, :], in0=cur[:, HALO:NCOL], scalar=1.0 / w,
                                                                                        in1=xn[:, c, HALO:NCOL], op0=ALU.mult, op1=ALU.subtract),
                              reads=[curk, ("xn", c)], writes=[("pooled", c)])
                        if g == 0:
                            p.add("dve", lambda e, c=c, cur=cur: e.tensor_tensor(out=tmpf[:, :], in0=cur[:, HALO:2 * HALO], in1=cfix[:, c, :], op=ALU.mult),
                                  reads=[curk, "cfix"], writes=["tmpf"])
                            p.add("dve", lambda e, c=c: e.tensor_tensor(out=pooled[:, c, 0:HALO], in0=tmpf[:, :], in1=xn[:, c, HALO:2 * HALO], op=ALU.subtract),
                                  reads=["tmpf", ("xn", c)], writes=[("pooled", c)])
                    sc = vec("pool_scale")
                    for oc in range(KC):
                        g4 = oc // 2
                        bk = cx.rot("poolb", 2)
                        for k in range(2):
                            p.add("pe", lambda e, oc=oc, g4=g4, k=k, bk=bk: e.matmul(cx.pb[bk][:, :], pw[:, g4, k, (oc % 2) * 128:(oc % 2 + 1) * 128],
                                                                                    pooled[:, 2 * g4 + k, :], start=(k == 0), stop=(k == 1)),
                                  reads=[("pw", g4), ("pooled", 2 * g4 + k)], writes=[("pb", bk)])
                        p.add("dve", lambda e, oc=oc, bk=bk: e.scalar_tensor_tensor(out=h[:, oc, t0:t0 + G], in0=cx.pb[bk][:, :], scalar=sc[:, oc:oc + 1],
                                                                                   in1=xh[:, oc, HALO:NCOL], op0=ALU.mult, op1=ALU.add),
                              reads=[("pb", bk), xk, "vecs"], writes=[("h", gl, oc)])
                if dbg and pp == 0:
                    p.dma("sp", d_h1, h[:, :, :], reads=[("h", gl, c) for gl in range(GP) for c in range(KC)], semname="d_dbg")
                    p.dma("sp", d_xn, xn[:, :, :], reads=[("xn", c) for c in range(KC)], semname="d_dbg2")
                    p.dma("sp", d_pooled, pooled[:, :, :], reads=[("pooled", c) for c in range(KC)], semname="d_dbg3")
                p.flush()
            es_ple = ExitStack()
            pleb = ple_alloc(cx, es_ple, GP, "e")
            with ExitStack() as es:
                for gl in range(GP):
                    rmsnorm_view(cx, h, hn, gl, gl * G, vec("ffn_norm0"), sq, rstd, "n")
                ffn_stage(cx, ffn_alloc(cx, es, "f"), h, hn, w1_d, w3_d, w2_d, D_FF, GP, "f")
                ple_load(cx, pleb, pT, pp * TP, pg_d, pp_d, GP, "e")
                p.flush()
            es_kv = ExitStack()
            wa = cx.sb(es_kv, "wa", [128, KC, D], BF16)
            wb = cx.sb(es_kv, "wb", [128, KC, D], BF16)
            with ExitStack() as es:
                for gl in range(GP):
                    rmsnorm_view(cx, h, hn, gl, gl * G, vec("ple_norm0"), sq, rstd, "n")
                ple_stage(cx, pleb, h, hn, GP, "e")
                load_w(cx, wa, ("wa",), wk_d, 0, D, KC)
                load_w(cx, wb, ("wb",), wv_d, 0, D, KC)
                p.flush()
            with ExitStack() as es:
                p.dma("sp", hT_o[:, :, pp * TP:(pp + 1) * TP], h[:, :, :], reads=[("h", gl, c) for gl in range(GP) for c in range(KC)],
                      semname="d_hout")
                rc = cx.sb(es, "rc", [32, TP], F32)
                rs = cx.sb(es, "rs", [32, TP], F32)
                kraw = [cx.sb(es, "kraw%d" % i, [128, G], BF16) for i in range(3)]
                vsb = [cx.sb(es, "vsb%d" % i, [128, D], BF16) for i in range(2)]
                t1 = cx.sb(es, "t1", [32, G], F32)
                t2 = cx.sb(es, "t2", [32, G], F32)
                p.dma("sp", rc[:, :], ropeC_d[:, pp * TP:(pp + 1) * TP], writes=["rc"])
                p.dma("sp", rs[:, :], ropeS_d[:, pp * TP:(pp + 1) * TP], writes=["rs"])
                for which in ("kv", "q"):
                    if which == "kv":
                        gname = "kv_norm"
                    else:
                        load_w(cx, wa, ("wa",), wq_d, 0, D, KC)
                        gname = "attn_norm"
                    for gl in range(GP):
                        rmsnorm_view(cx, h, hn, gl, gl * G, vec(gname), sq, rstd, "n")
                    kunits = [(gl, hd) for gl in range(GP) for hd in range(KC)]

                    def emitA(k):
                        gl, hd = kunits[k]
                        t0 = gl * G
                        bk = (0, 1, 5)[k % 3]
                        ks = k % 3
                        for c in range(KC):
                            p.add("pe", lambda e: e.matmul(cx.pb[bk][:, :], wa[:, c, hd * 128:(hd + 1) * 128], hn[:, c, t0:t0 + G],
                                                           start=(c == 0), stop=(c == KC - 1)),
                                  reads=[("wa",), ("hn", gl, c)], writes=[("pb", bk)])
                        p.add("act", lambda e: e.activation(out=kraw[ks][:, :], in_=cx.pb[bk][:, :], func=AF.Copy),
                              reads=[("pb", bk)], writes=[("kraw", ks)])

                    def emitB(k):
                        gl, hd = kunits[k]
                        g = pp * GP + gl
                        t0 = gl * G
                        ks = k % 3
                        p.add("pe", lambda e: e.matmul(cx.pb[2][0:32, :], cx.rsw[:, :], kraw[ks][0:32, :], start=True, stop=True),
                              reads=[("kraw", ks), "rsw"], writes=[("pb", 2)])
                        p.add("dve", lambda e: e.tensor_tensor(out=t1[:, :], in0=kraw[ks][0:32, :], in1=rc[:, t0:t0 + G], op=ALU.mult),
                              reads=[("kraw", ks), "rc"], writes=["t1"])
                        p.add("dve", lambda e: e.tensor_tensor(out=t2[:, :], in0=cx.pb[2][0:32, :], in1=rs[:, t0:t0 + G], op=ALU.mult),
                              reads=[("pb", 2), "rs"], writes=["t2"])
                        p.add("dve", lambda e: e.tensor_tensor(out=kraw[ks][0:32, :], in0=t1[:, :], in1=t2[:, :], op=ALU.add),
                              reads=["t1", "t2"], writes=[("kraw", ks)])
                        if which == "kv":
                            p.add("dve", lambda e: e.reduce_sum(out=kmT[:, hd, 2 * g:2 * g + 2],
                                                                in_=kraw[ks][:, :].rearrange("p (a b) -> p a b", b=256), axis=AX.X),
                                  reads=[("kraw", ks)], writes=["kmT"])
                        kdst = KT_s[hd][:, g * G:(g + 1) * G] if which == "kv" else QT_o[:, hd, g * G:(g + 1) * G]
                        p.dma("sp", kdst, kraw[ks][:, :], reads=[("kraw", ks)], semname="d_kout%d" % ks)

                    emitA(0)
                    for k in range(len(kunits)):
                        if k + 1 < len(kunits):
                            emitA(k + 1)
                        emitB(k)
                    for gl in range(GP):
                        g = pp * GP + gl
                        t0 = gl * G
                        if which == "kv":
                            for tcn in range(G // 128):
                                vs = cx.rot("vsb", 2)
                                for half in range(2):
                                    bk = 3 + cx.rot("vb", 2)
                                    for c in range(KC):
                                        p.add("pe", lambda e, c=c, half=half, bk=bk, tcn=tcn: e.matmul(
                                            cx.pb[bk][:, :], hn[:, c, t0 + tcn * 128:t0 + (tcn + 1) * 128], wb[:, c, half * 512:(half + 1) * 512],
                                            start=(c == 0), stop=(c == KC - 1)),
                                            reads=[("wb",), ("hn", gl, c)], writes=[("pb", bk)])
                                    p.add("act", lambda e, bk=bk, vs=vs, half=half: e.activation(out=vsb[vs][:, half * 512:(half + 1) * 512], in_=cx.pb[bk][:, :], func=AF.Copy),
                                          reads=[("pb", bk)], writes=[("vsb", vs, half)])
                                ktg = g * 4 + tcn
                                for hh in range(KC):
                                    p.dma("sp", V_s[hh][:, ktg * 128:(ktg + 1) * 128], vsb[vs][:, hh * 128:(hh + 1) * 128],
                                          reads=[("vsb", vs, hh // 4)], semname="d_vout%d_%d" % (vs, hh // 4))
                p.flush()
            es_kv.close()
            es_ple.close()
        p.dma("sp", km_o.rearrange("p (h b) -> p h b", h=KC), kmT[:, :, :], reads=["kmT"], semname="d_kmout")
        p.flush()


BIG = 30000.0
ATT_SCALE = 128.0 ** -0.5


def attention_phase(cx, QT_d, Kg, Vg, kmg, esel_d, ident_d, gb_d, valid_d, own_d, caus_d, OT_d, cc_keys):
    p = cx.p
    with ExitStack() as es:
        kmT = cx.sb(es, "kmT", [128, KC, NB], BF16)
        ident = cx.sb(es, "ident", [128, 128], BF16)
        esel = cx.sb(es, "esel", [128, NB * 128], BF16)
        gb = cx.sb(es, "gb", [128, 32, NB], BF16)
        valid = cx.sb(es, "valid", [128, 32, NB], BF16)
        own = cx.sb(es, "own", [128, 32, NB], BF16)
        caus = cx.sb(es, "caus", [128, 4, 4, G], BF16)
        qh = [cx.sb(es, "qh%d" % i, [128, TC], BF16) for i in range(2)]
        kh = cx.sb(es, "kh", [128, S], BF16)
        vh = cx.sb(es, "vh", [128, S // 128, 128], BF16)
        mbT = [cx.sb(es, "mbT%d" % i, [128, TC], BF16) for i in range(2)]
        oh = [cx.sb(es, "oh%d" % i, [128, TC], BF16) for i in range(2)]
        pT = [cx.sb(es, "pT%d" % i, [128, G], BF16) for i in range(6)]
        gm = [cx.sb(es, "gm%d" % i, [128, NB], F32) for i in range(2)]
        m8 = [cx.sb(es, "m8%d" % i, [128, 8], F32) for i in range(2)]
        mbq = [cx.sb(es, "mbq%d" % i, [128, NB], BF16) for i in range(2)]
        rden = cx.sb(es, "rden", [128, G], F32)
        for r4 in range(4):
            p.dma("pool", kmT[:, :, r4 * 16:(r4 + 1) * 16], kmg[r4 * 128:(r4 + 1) * 128, :].rearrange("p (h b) -> p h b", h=KC),
                  reads=[cc_keys["km"]], writes=[("kmT", r4)], semname="d_kmT")
        p.dma("pool", ident[:, :], ident_d, writes=["ident"])
        p.dma("pool", esel[:, :], esel_d, writes=["esel"])
        for i2_ in range(2):
            p.add("dve", lambda e: e.memset(mbT[i2_][64:128, :], 0.0), writes=[("mbTz", i2_)])
        p.dma("pool", gb[:, :, :], gb_d, writes=["gb"])
        p.dma("pool", valid[:, :, :], valid_d, writes=["valid"])
        p.dma("pool", own[:, :, :], own_d, writes=["own"])
        p.dma("pool", caus[:, :, :, :], caus_d, writes=["caus"])
        NCH = 4
        accs = {"pool": [cx.sb(es, "accp%d" % i, [128, G], F32) for i in range(2)],
                "dve": [cx.sb(es, "accd%d" % i, [128, G], F32) for i in range(2)]}
        ones32 = cx.sb(es, "ones32", [128, 128], F32)
        p.add("dve", lambda e: e.memset(ones32[:, :], 1.0), writes=["ones32"])

        def gating_steps(hd):
            hs = hd % 2
            steps = []

            def mkA(qc):
                def step():
                    gs = qc % 2
                    p.add("pe", lambda e: e.matmul(cx.pb[6][:, :NB], qh[hs][:, qc * 128:(qc + 1) * 128], kmT[:, hd, :], start=True, stop=True),
                          reads=[("qh", hs)] + [("kmT", r4) for r4 in range(4)], writes=[("pb", 6)])
                    p.add("dve", lambda e: e.tensor_tensor(out=gm[gs][:, :], in0=cx.pb[6][:, :NB], in1=gb[:, qc, :], op=ALU.add),
                          reads=[("pb", 6), "gb"], writes=[("gm", gs)])
                    p.add("dve", lambda e: e.max(out=m8[gs][:, :], in_=gm[gs][:, :]), reads=[("gm", gs)], writes=[("m8", gs)])
                    p.add("dve", lambda e: e.tensor_scalar(gm[gs][:, :], gm[gs][:, :], m8[gs][:, 2:3], None, ALU.is_ge),
                          reads=[("gm", gs), ("m8", gs)], writes=[("gm", gs)])
                    p.add("dve", lambda e: e.tensor_tensor(out=gm[gs][:, :], in0=gm[gs][:, :], in1=valid[:, qc, :], op=ALU.mult),
                          reads=[("gm", gs), "valid"], writes=[("gm", gs)])
                    p.add("dve", lambda e: e.tensor_tensor(out=gm[gs][:, :], in0=gm[gs][:, :], in1=own[:, qc, :], op=ALU.add),
                          reads=[("gm", gs), "own"], writes=[("gm", gs)])
                    p.add("dve", lambda e: e.tensor_scalar(mbq[gs][:, :], gm[gs][:, :], 1.0, BIG, ALU.subtract, ALU.mult),
                          reads=[("gm", gs)], writes=[("mbq", gs)])
                return step

            def mkB(qc):
                def step():
                    gs = qc % 2
                    p.add("pe", lambda e: e.matmul(cx.pb[7][0:NB, 0:128], mbq[gs][:, :], ident[:, :], start=True, stop=True),
                          reads=[("mbq", gs), "ident"], writes=[("pb", 7)])
                    p.add("act", lambda e: e.activation(out=mbT[hs][0:NB, qc * 128:(qc + 1) * 128], in_=cx.pb[7][0:NB, 0:128], func=AF.Copy),
                          reads=[("pb", 7)], writes=[("mbT", hs, qc // 4)])
                return step
            nq = TC // 128
            for qc in range(nq + 1):
                if qc < nq:
                    steps.append(mkA(qc))
                if qc >= 1:
                    steps.append(mkB(qc - 1))
            return steps

        p.dma("sp", qh[0][:, :], QT_d[:, 0, :], writes=[("qh", 0)])
        for st_ in gating_steps(0):
            st_()
        def load_kv(hd):
            for c in range(NCH):
                w = S // NCH
                p.dma("sp", kh[:, c * w:(c + 1) * w], Kg[hd][c * 128:(c + 1) * 128, :], reads=[cc_keys["k", hd]], writes=[("kh", c)])
                wt = (S // 128) // NCH
                p.dma("sp", vh[:, c * wt:(c + 1) * wt, :], Vg[hd][c * 128:(c + 1) * 128, :].rearrange("p (k d) -> p k d", d=128),
                      reads=[cc_keys["v", hd]], writes=[("vh", c)])

        load_kv(0)
        for hd in range(KC):
            hs = hd % 2
            nxt = []
            if hd + 1 < KC:
                p.dma("sp", qh[1 - hs][:, :], QT_d[:, hd + 1, :], writes=[("qh", 1 - hs)])
                nxt = gating_steps(hd + 1)
            for i in range(NGRP):
                tiles = [(r4, i2, t4) for r4 in range(4) for i2 in range(i + 1) for t4 in range(4)]
                ntile = len(tiles)
                q0 = i * G
                LOOK = 3
                slots = {}
                asl = i % 2
                ob_ = 4 + (i % 2)
                for step in range(ntile + LOOK):
                    if step < ntile:
                        r4, i2, t4 = tiles[step]
                        kt = r4 * 32 + i2 * 4 + t4
                        sb_ = cx.rot("S", 4)
                        ps_ = cx.rot("pT", 6)
                        slots[step] = (ps_, kt)
                        n = r4 * 16 + i2 * 2 + t4 // 2
                        own_pair = (i2 == i)
                        kc_ = r4
                        p.add("pe", lambda e: e.matmul(cx.pb[sb_][:, :], kh[:, kt * 128:(kt + 1) * 128], qh[hs][:, q0:q0 + G], start=True, stop=False),
                              reads=[("kh", kc_), ("qh", hs)], writes=[("pb", sb_)])
                        p.add("pe", lambda e: e.matmul(cx.pb[sb_][:, :], esel[:, n * 128:(n + 1) * 128], mbT[hs][:, q0:q0 + G], start=False, stop=not own_pair),
                              reads=["esel", ("mbT", hs, i), ("mbTz", hs)], writes=[("pb", sb_)])
                        if own_pair:
                            rr, kr = r4, t4
                            p.add("pe", lambda e: e.matmul(cx.pb[sb_][:, :], ident[:, :], caus[:, rr, kr, :], start=False, stop=True),
                                  reads=["ident", "caus"], writes=[("pb", sb_)])
                        p.add("act", lambda e: e.activation(out=pT[ps_][:, :], in_=cx.pb[sb_][:, :], func=AF.Exp, scale=ATT_SCALE),
                              reads=[("pb", sb_)], writes=[("pT", ps_)])
                        if nxt and i >= NGRP - 4 and step % 6 == 1:
                            nxt.pop(0)()
                    kv = step - LOOK
                    if kv >= 0:
                        ps_, ktv = slots.pop(kv)
                        vc_ = ktv // 32
                        p.add("pe", lambda e: e.matmul(cx.pb[ob_][:, :], vh[:, ktv, :], pT[ps_][:, :], start=(kv == 0), stop=(kv == ntile - 1)),
                              reads=[("vh", vc_), ("pT", ps_)], writes=[("pb", ob_)])
                        en = "pool" if kv % 2 == 0 else "dve"
                        acc = accs[en][asl]
                        if kv < 2:
                            p.add(en, lambda e: e.tensor_copy(out=acc[:, :], in_=pT[ps_][:, :]), reads=[("pT", ps_)], writes=[("acc", en, asl)])
                        else:
                            p.add(en, lambda e: e.tensor_tensor(out=acc[:, :], in0=acc[:, :], in1=pT[ps_][:, :], op=ALU.add),
                                  reads=[("pT", ps_), ("acc", en, asl)], writes=[("acc", en, asl)])
                p.add("pe", lambda e: e.matmul(cx.pb[7][:, :], ones32[:, :], accs["pool"][asl][:, :], start=True, stop=False),
                      reads=["ones32", ("acc", "pool", asl)], writes=[("pb", 7)])
                p.add("pe", lambda e: e.matmul(cx.pb[7][:, :], ones32[:, :], accs["dve"][asl][:, :], start=False, stop=True),
                      reads=["ones32", ("acc", "dve", asl)], writes=[("pb", 7)])
                p.add("dve", lambda e: e.reciprocal(out=rden[:, :], in_=cx.pb[7][:, :]), reads=[("pb", 7)], writes=["rden"])
                p.add("dve", lambda e: e.tensor_tensor(out=oh[hs][:, q0:q0 + G], in0=cx.pb[ob_][:, :], in1=rden[:, :], op=ALU.mult),
                      reads=[("pb", ob_), "rden"], writes=[("oh", hs)])
            while nxt:
                nxt.pop(0)()
            if hd + 1 < KC:
                load_kv(hd + 1)
            p.dma("sp", OT_d[:, hd, :], oh[hs][:, :], reads=[("oh", hs)], semname="d_ohout%d" % hs)
        p.flush()


def emit_l2(cx, A, npass=NPASS, nexp=NEXP):
    dbg = False
    p = cx.p
    hT_d, pT, ident_d = A["hscr"], A["pT1"], A["ident"]
    wo_d, router_d, e1_d, e3_d, e2_d, pg_d, pp_d = A["w_o"], A["router"], A["exp_w1"], A["exp_w3"], A["exp_w2"], A["ple_gate1"], A["ple_proj1"]
    out_o, OT_d = A["outT"], A["OTscr"]
    vec = lambda n: cx.vecs[:, VI[n], :]
    if True:
        attention_phase(cx, A["QTscr"], A["Kg"], A["Vg"], A["kmg"], A["esel"], ident_d, A["gb"], A["valid"], A["own"], A["caus"], OT_d, A["cc_keys"])
        with ExitStack() as es1:
            h = cx.sb(es1, "h", [128, KC, TP], F32)
            hn = cx.sb(es1, "hn", [128, KC, TP], BF16)
            sq = cx.sb(es1, "sq", [128, KC, G], BF16)
            rstd = cx.sb(es1, "rstd", [128, G], F32)
            for pp in range(npass):
                tok0 = pp * TP
                es_moe = ExitStack()
                bufs = ffn_alloc(cx, es_moe, "m")
                with ExitStack() as es:
                    wo = cx.sb(es, "wo", [128, KC, D], BF16)
                    load_w(cx, wo, ("wo",), wo_d, 0, D, KC)
                    p.dma("sp", h[:, :, :], hT_d[:, :, tok0:tok0 + TP], writes=[("h", gl, c) for gl in range(GP) for c in range(KC)])
                    p.dma("sp", hn[:, :, :], OT_d[:, :, tok0:tok0 + TP], writes=[("hn", gl, c) for gl in range(GP) for c in range(KC)])
                    for gl in range(GP):
                        t0 = gl * G
                        for oc in range(KC):
                            bk = cx.rot("wob", 3)
                            for c in range(KC):
                                p.add("pe", lambda e: e.matmul(cx.pb[bk][:, :], wo[:, c, oc * 128:(oc + 1) * 128], hn[:, c, t0:t0 + G],
                                                               start=(c == 0), stop=(c == KC - 1)),
                                      reads=[("wo",), ("hn", gl, c)], writes=[("pb", bk)])
                            p.add("dve", lambda e: e.tensor_tensor(out=h[:, oc, t0:t0 + G], in0=h[:, oc, t0:t0 + G], in1=cx.pb[bk][:, :], op=ALU.add),
                                  reads=[("pb", bk), ("h", gl, oc)], writes=[("h", gl, oc)])
                    ffn_prefetch(cx, bufs, e1_d[0], e3_d[0], e2_d[0], E_FF, "m")
                    p.flush()
                with ExitStack() as es:
                    rt = cx.sb(es, "rt", [128, KC, NEXP], BF16)
                    ident = cx.sb(es, "identb", [128, 128], BF16)
                    lg = cx.sb(es, "lg", [128, NEXP], F32)
                    m8 = cx.sb(es, "m8r", [128, 8], F32)
                    nm1 = cx.sb(es, "nm1", [128, 1], F32)
                    ex = cx.sb(es, "ex", [128, NEXP], F32)
                    den = cx.sb(es, "den", [128, 1], F32)
                    gates = cx.sb(es, "gates", [128, GP * 4, NEXP], F32)
                    dg = [cx.sb(es, "dg%d" % i, [128, 128], BF16) for i in range(2)]
                    gbcs = [cx.sb(es, "gbc%d" % i, [128, GP, G], BF16) for i in range(2)]
                    load_w(cx, rt, ("rt",), router_d, 0, NEXP, KC)
                    p.dma("pool", ident[:, :], ident_d, writes=["identb"])
                    for gl in range(GP):
                        rmsnorm_view(cx, h, hn, gl, gl * G, vec("ffn_norm1"), sq, rstd, "n")
                    for tcn in range(GP * 4):
                        gl = tcn // 4
                        for c in range(KC):
                            p.add("pe", lambda e: e.matmul(cx.pb[6][:, :NEXP], hn[:, c, tcn * 128:(tcn + 1) * 128], rt[:, c, :], start=(c == 0), stop=(c == KC - 1)),
                                  reads=[("rt",), ("hn", gl, c)], writes=[("pb", 6)])
                        p.add("dve", lambda e: e.tensor_copy(out=lg[:, :], in_=cx.pb[6][:, :NEXP]), reads=[("pb", 6)], writes=["lg"])
                        p.add("dve", lambda e: e.max(out=m8[:, :], in_=lg[:, :]), reads=["lg"], writes=["m8r"])
                        p.add("dve", lambda e: e.tensor_scalar(nm1[:, :], m8[:, 0:1], -1.0, None, ALU.mult), reads=["m8r"], writes=["nm1"])
                        p.add("act", lambda e: e.activation(out=ex[:, :], in_=lg[:, :], func=AF.Exp, bias=nm1[:, 0:1], scale=1.0),
                              reads=["lg", "nm1"], writes=["ex"])
                        p.add("dve", lambda e: e.tensor_scalar(lg[:, :], lg[:, :], m8[:, 1:2], None, ALU.is_ge), reads=["lg", "m8r"], writes=["lg"])
                        p.add("dve", lambda e: e.tensor_tensor(out=ex[:, :], in0=ex[:, :], in1=lg[:, :], op=ALU.mult), reads=["ex", "lg"], writes=["ex"])
                        p.add("dve", lambda e: e.reduce_sum(out=den[:, :], in_=ex[:, :], axis=AX.X), reads=["ex"], writes=["den"])
                        p.add("dve", lambda e: e.reciprocal(out=den[:, :], in_=den[:, :]), reads=["den"], writes=["den"])
                        p.add("dve", lambda e: e.tensor_scalar(gates[:, tcn, :], ex[:, :], den[:, 0:1], None, ALU.mult), reads=["ex", "den"], writes=[("gates", tcn)])
                    if dbg and pp == 0:
                        p.dma("sp", d_gates, gates[:, :, :], reads=[("gates", t) for t in range(GP * 4)], semname="d_dbg5")
                    for ex_i in range(nexp):
                        gbc = gbcs[ex_i % 2]
                        gkey = ("gbc", ex_i % 2)
                        for gl in range(GP):
                            for t4 in range(4):
                                tcn = gl * 4 + t4
                                ds = cx.rot("dg", 2)
                                p.add("dve", lambda e: e.tensor_scalar(dg[ds][:, :], ident[:, :], gates[:, tcn, ex_i:ex_i + 1], None, ALU.mult),
                                      reads=["identb", ("gates", tcn)], writes=[("dg", ds)])
                                p.add("pe", lambda e: e.matmul(cx.pb[6][:, t4 * 128:(t4 + 1) * 128], cx.ones[:, :], dg[ds][:, :], start=True, stop=True),
                                      reads=["ones", ("dg", ds)], writes=[("pb", 6, t4)])
                            p.add("act", lambda e: e.activation(out=gbc[:, gl, :], in_=cx.pb[6][:, :], func=AF.Copy),
                                  reads=[("pb", 6, t4) for t4 in range(4)], writes=[gkey + (gl,)])
                        ffn_stage(cx, bufs, h, hn, e1_d[ex_i], e3_d[ex_i], e2_d[ex_i], E_FF, GP, "m", gate=(gbc, gkey))
                    p.flush()
                es_moe.close()
                with ExitStack() as es:
                    for gl in range(GP):
                        rmsnorm_view(cx, h, hn, gl, gl * G, vec("ple_norm1"), sq, rstd, "n")
                    pleb = ple_alloc(cx, es, GP, "e")
                    ple_load(cx, pleb, pT, tok0, pg_d, pp_d, GP, "e")
                    ple_stage(cx, pleb, h, hn, GP, "e")
                    p.flush()
                with ExitStack() as es:
                    ob = [cx.sb(es, "ob%d" % i, [128, KC, G], F32) for i in range(2)]
                    for gl in range(GP):
                        os_ = gl % 2
                        rmsnorm_view(cx, h, ob[os_], gl, gl * G, vec("final_norm"), sq, rstd, "n", dst_local=True, dkey=("ob", os_))
                        p.dma("sp", out_o[:, :, tok0 + gl * G:tok0 + (gl + 1) * G], ob[os_][:, :, :], reads=[("ob", os_)], semname="d_out%d" % os_)
                    p.flush()


def core_positions(r):
    return np.concatenate([np.arange(512 * (r + 4 * i), 512 * (r + 4 * i) + 512) for i in range(NGRP)])


def fm(a):
    T, nf = a.shape
    return np.ascontiguousarray(a.reshape(T, nf // 128, 128).transpose(2, 1, 0))


def vec_fm(v):
    return np.ascontiguousarray(v.reshape(KC, 128).T)


def rope_tables(pos):
    half = 16
    inv_freq = np.float32(500000.0) ** (-(np.arange(0, 32, 2, dtype=np.float32) / np.float32(32)))
    ang = pos.astype(np.float32)[None, :] * inv_freq[:, None].astype(np.float32)
    c = np.cos(ang).astype(np.float32)
    s = np.sin(ang).astype(np.float32)
    return np.concatenate([c, c], 0), np.concatenate([-s, s], 0)


def build(npass=NPASS, nexp=NEXP):
    nc = bass.Bass("TRN2", target_bir_lowering=False)

    def din(name, shape, dt=F32):
        return nc.dram_tensor(name, list(shape), dt, kind="ExternalInput").ap()

    def scr(name, shape, dt):
        return nc.dram_tensor(name, list(shape), dt)

    A = dict(
        xT=din("xT", [128, KC, NGRP, G + HALO]), pT0=din("pT0", [128, 2, TC]), pT1=din("pT1", [128, 2, TC]),
        cntfix=din("cntfix", [128, KC, HALO]), ropeC=din("ropeC", [32, TC]), ropeS=din("ropeS", [32, TC]),
        pool_w=din("pool_w", [4, 256, 256]), ffn_w1=din("ffn_w1", [D, D_FF]), ffn_w3=din("ffn_w3", [D, D_FF]), ffn_w2=din("ffn_w2", [D_FF, D]),
        ple_gate0=din("ple_gate0", [D, D]), ple_proj0=din("ple_proj0", [PLE, D]), ple_gate1=din("ple_gate1", [D, D]), ple_proj1=din("ple_proj1", [PLE, D]),
        w_k=din("w_k", [D, D]), w_v=din("w_v", [D, D]), w_q=din("w_q", [D, D]), w_o=din("w_o", [D, D]),
        router=din("router", [D, NEXP]), exp_w1=din("exp_w1", [NEXP, D, E_FF]), exp_w3=din("exp_w3", [NEXP, D, E_FF]), exp_w2=din("exp_w2", [NEXP, E_FF, D]),
        esel=din("esel", [128, NB * 128]), ident=din("ident", [128, 128]), gb=din("gb", [128, 32, NB]), valid=din("valid", [128, 32, NB]),
        own=din("own", [128, 32, NB]), caus=din("caus", [128, 4, 4, G]),
    )
    vecs_d = din("vecs", [128, len(VEC_NAMES), KC])
    rsw_d = din("rsw", [32, 32])
    A["outT"] = nc.dram_tensor("outT", [128, KC, TC], F32, kind="ExternalOutput").ap()
    A["hscr"] = scr("hscr", [128, KC, TC], F32).ap()
    A["QTscr"] = scr("QTscr", [128, KC, TC], BF16).ap()
    A["OTscr"] = scr("OTscr", [128, KC, TC], BF16).ap()
    kts = [scr("KTscr%d" % i, [128, TC], BF16) for i in range(KC)]
    vss = [scr("Vscr%d" % i, [128, TC], BF16) for i in range(KC)]
    kgs = [scr("Kg%d" % i, [4 * 128, TC], BF16) for i in range(KC)]
    vgs = [scr("Vg%d" % i, [4 * 128, TC], BF16) for i in range(KC)]
    kms = scr("kmscr", [128, KC * 16], F32)
    kmg = scr("kmg", [4 * 128, KC * 16], F32)
    A["KTscr"] = [t.ap() for t in kts]
    A["Vscr"] = [t.ap() for t in vss]
    A["Kg"] = [t.ap() for t in kgs]
    A["Vg"] = [t.ap() for t in vgs]
    A["kmscr"] = kms.ap()
    A["kmg"] = kmg.ap()
    groups = [[0, 1, 2, 3], [4, 5, 6, 7]]
    with ExitStack() as es0:
        cx = Ctx(nc, es0)
        p = cx.p
        common_consts(cx, es0, vecs_d, rsw_d)
        p.flush()
        emit_l1(cx, A, npass)
        cck = {"km": ("cc", "km")}
        p.cc(kms.ap().opt(), kmg.ap().opt(), groups, reads=[], writes=[cck["km"]], semname="cc_km")
        for hd in range(KC):
            cck["k", hd] = ("cc", "k", hd)
            cck["v", hd] = ("cc", "v", hd)
            p.cc(kts[hd].ap().opt(), kgs[hd].ap().opt(), groups, reads=[], writes=[cck["k", hd]], semname="cc_k%d" % hd)
            p.cc(vss[hd].ap().opt(), vgs[hd].ap().opt(), groups, reads=[], writes=[cck["v", hd]], semname="cc_v%d" % hd)
        A["cc_keys"] = cck
        emit_l2(cx, A, npass, nexp)
    return nc


def seq_block_of(n2):
    r4, rem = n2 // 16, n2 % 16
    return 2 * (r4 + 4 * (rem // 2)) + rem % 2


def l2_consts(r):
    gb = np.zeros((128, 32, NB), np.float32)
    valid = np.zeros((128, 32, NB), np.float32)
    own = np.zeros((128, 32, NB), np.float32)
    nseq = np.array([seq_block_of(n2) for n2 in range(NB)])
    for qc in range(32):
        i = qc // 4
        j = 2 * (r + 4 * i) + (qc % 4) // 2
        gb[:, qc, nseq >= j] = -1e30
        valid[:, qc, nseq < j] = 1.0
        own[:, qc, nseq == j] = 1.0
    caus = np.zeros((128, 4, 4, G), np.float32)
    pk = np.arange(128)[:, None]
    t = np.arange(G)[None, :]
    for kr in range(4):
        same = (kr // 2) == (t // 256)
        fut = ((kr % 2) * 128 + pk) > (t % 256)
        caus[:, r, kr, :] = np.where(same & fut, -BIG, 0.0)
    return gb, valid, own, caus


def make_inputs(inp):
    x, p = inp["x"], inp["p"]
    vecs = np.stack([vec_fm(v) for v in (inp["pool_norm"][0], inp["pool_scale"][0], inp["ffn_norm"][0], inp["ple_norm"][0], inp["kv_norm"],
                                         inp["attn_norm"][0], inp["ffn_norm"][1], inp["ple_norm"][1], inp["final_norm"])], axis=1)
    rsw = np.zeros((32, 32), np.float32)
    for i in range(16):
        rsw[16 + i, i] = 1.0
        rsw[i, 16 + i] = 1.0
    esel = np.zeros((128, NB * 128), np.float32)
    for n in range(NB):
        esel[n, n * 128:(n + 1) * 128] = 1.0
    shared = dict(vecs=np.ascontiguousarray(vecs, dtype=np.float32), rsw=rsw, pool_w=np.ascontiguousarray(inp["pool_w"][0]),
                  ffn_w1=inp["ffn_w1"][0], ffn_w3=inp["ffn_w3"][0], ffn_w2=inp["ffn_w2"][0], ple_gate0=inp["ple_gate"][0],
                  ple_proj0=inp["ple_proj"][0], ple_gate1=inp["ple_gate"][1], ple_proj1=inp["ple_proj"][1],
                  w_k=inp["w_k"], w_v=inp["w_v"], w_q=inp["w_q"][0], w_o=inp["w_o"][0], router=inp["router"][0],
                  exp_w1=inp["exp_w1"][0], exp_w3=inp["exp_w3"][0], exp_w2=inp["exp_w2"][0], esel=esel, ident=np.eye(128, dtype=np.float32))
    maps = []
    for c in range(8):
        b, r = c // 4, c % 4
        pos = core_positions(r)
        xT = np.zeros((128, KC, NGRP, G + HALO), np.float32)
        for i in range(NGRP):
            s0 = 512 * (r + 4 * i)
            lo = max(s0 - HALO, 0)
            seg = x[b, lo:s0 + G]
            xT[:, :, i, G + HALO - seg.shape[0]:] = seg.reshape(-1, KC, 128).transpose(2, 1, 0)
        cntfix = np.zeros((128, KC, HALO), np.float32)
        for cch in range(KC):
            w = POOL_W[cch // 2]
            if r == 0:
                cntfix[:, cch, :] = 1.0 / np.minimum(np.arange(HALO) + 1, w).astype(np.float32)
            else:
                cntfix[:, cch, :] = 1.0 / w
        rc, rs = rope_tables(pos)
        gb, valid, own, caus = l2_consts(r)
        m = dict(shared)
        m.update(xT=xT, pT0=fm(p[0, b][pos]), pT1=fm(p[1, b][pos]), cntfix=cntfix, ropeC=rc, ropeS=rs, gb=gb, valid=valid, own=own, caus=caus)
        maps.append(m)
    return maps


def kernel(**inputs):
    inp = {k: np.asarray(v) for k, v in inputs.items()}
    nc = build()
    maps = make_inputs(inp)
    res = run_bass_kernel_spmd(nc, maps, core_ids=list(range(8)))
    out = np.zeros((B, S, D), np.float32)
    for c in range(8):
        b, r = c // 4, c % 4
        pos = core_positions(r)
        out[b, pos] = np.asarray(res.results[c]["outT"]).transpose(2, 1, 0).reshape(TC, D)
    return out
```

```python
import numpy as np
from contextlib import ExitStack
import concourse.bass as bass
import concourse.mybir as mybir
from concourse.bass_utils import run_bass_kernel_spmd

F32 = mybir.dt.float32
BF16 = mybir.dt.bfloat16
AF = mybir.ActivationFunctionType
ALU = mybir.AluOpType
AX = mybir.AxisListType

D = 1024
KC = 8
S = 16384
B = 2
G = 512
NGRP = 8
TC = G * NGRP
HALO = 16
GP = 4
NPASS = NGRP // GP
TP = GP * G
D_FF = 2816
E_FF = 3584
NEXP = 8
PLE = 256
EPS = 1e-6
NB = S // 256
ENGS = ("pe", "act", "dve", "pool", "sp")


class _Op:
    __slots__ = ("eng", "fn", "reads", "writes", "dma", "semname", "deps", "signal", "sem", "cnt", "inc")


class _Rec:
    def __init__(self):
        self.call = None

    def __getattr__(self, name):
        def f(*a, **kw):
            self.call = (name, a, kw)
            return self
        return f


class Prog:
    def __init__(self, nc):
        self.nc = nc
        self.ops = []
        self.last_w = {}
        self.readers = {}
        self.eng_sem = {e: nc.alloc_semaphore(name="s_" + e) for e in ENGS}
        self.eng_cnt = {e: 0 for e in ENGS}
        self.dma_sem = {}
        self.dma_cnt = {}
        self.waited = {e: {} for e in ENGS}

    def add(self, eng, fn, reads=(), writes=(), dma=False, semname=None):
        op = _Op()
        rec = _Rec()
        fn(rec)
        op.eng, op.fn, op.reads, op.writes, op.dma = eng, rec.call, tuple(reads), tuple(writes), dma
        op.semname = semname
        op.deps, op.signal, op.sem, op.cnt, op.inc = [], dma, None, 0, 16
        self.ops.append(op)
        return op

    def cc(self, ins_ap, outs_ap, groups, reads, writes, semname):
        op = self.add("pool", lambda e: e.collective_compute("AllGather", ALU.bypass, replica_groups=groups, ins=[ins_ap], outs=[outs_ap]),
                      reads, writes, True, semname)
        op.inc = 1
        return op

    def dma(self, eng, out, in_, reads=(), writes=(), semname=None):
        if semname is None:
            k = writes[0] if writes else reads[0]
            semname = "d_" + "_".join(str(x) for x in k)
        return self.add(eng, lambda e: e.dma_start(out=out, in_=in_), reads, writes, True, semname)

    def flush(self, barrier=True):
        nc = self.nc
        ops, self.ops = self.ops, []
        for op in ops:
            deps = {}
            for k in op.reads:
                w = self.last_w.get(k)
                if w is not None:
                    deps[id(w)] = w
            for k in op.writes:
                w = self.last_w.get(k)
                if w is not None:
                    deps[id(w)] = w
                for r in self.readers.get(k, ()):
                    deps[id(r)] = r
            for k in op.reads:
                self.readers.setdefault(k, []).append(op)
            for k in op.writes:
                self.last_w[k] = op
                self.readers[k] = []
            for d in deps.values():
                if d is op:
                    continue
                if d.eng == "pe" and op.eng == "pe" and not d.dma and not op.dma:
                    continue
                op.deps.append(d)
                d.signal = True
        for op in ops:
            if not op.signal:
                continue
            if op.dma:
                if op.semname not in self.dma_sem:
                    self.dma_sem[op.semname] = nc.alloc_semaphore(name=op.semname[:40])
                    self.dma_cnt[op.semname] = 0
                self.dma_cnt[op.semname] += op.inc
                op.sem, op.cnt = op.semname, self.dma_cnt[op.semname]
            else:
                self.eng_cnt[op.eng] += 1
                op.sem, op.cnt = op.eng, self.eng_cnt[op.eng]
        per = {e: [o for o in ops if o.eng == e] for e in ENGS}

        def semh(name):
            return self.eng_sem[name] if name in self.eng_sem else self.dma_sem[name]

        def emit(e, eng):
            wd = self.waited[e]
            for op in per[e]:
                need = {}
                for d in op.deps:
                    if need.get(d.sem, 0) < d.cnt:
                        need[d.sem] = d.cnt
                for s, c in need.items():
                    if wd.get(s, 0) < c:
                        eng.wait_ge(semh(s), c)
                        wd[s] = c
                ins = getattr(eng, op.fn[0])(*op.fn[1], **op.fn[2])
                if op.signal:
                    if op.dma:
                        ins.then_inc(self.dma_sem[op.sem], op.inc)
                    else:
                        ins.then_inc(self.eng_sem[e], 1)
            if e == "sp":
                for s, c in self.dma_cnt.items():
                    if wd.get(s, 0) < c:
                        eng.wait_ge(self.dma_sem[s], c)
                        wd[s] = c

        with nc.Block() as block:
            block.tensor(lambda t: emit("pe", t))
            block.scalar(lambda t: emit("act", t))
            block.vector(lambda t: emit("dve", t))
            block.gpsimd(lambda t: emit("pool", t))
            block.sync(lambda t: emit("sp", t))
        if barrier:
            nc.all_engine_barrier()
            self.last_w = {}
            self.readers = {}


class Ctx:
    def __init__(self, nc, es):
        self.nc = nc
        self.es = es
        self.p = Prog(nc)
        self.pb = [es.enter_context(nc.psum_tensor("pb%d" % i, [128, 512], F32)) for i in range(8)]
        self.rr = {}

    def sb(self, es, name, shape, dt):
        self.uid = getattr(self, "uid", 0) + 1
        return es.enter_context(self.nc.sbuf_tensor("sb%d_%s" % (self.uid, name), shape, dt))

    def rot(self, name, n):
        v = self.rr.get(name, 0)
        self.rr[name] = v + 1
        return v % n


def load_w(cx, dst, dkey, w_ap, c0, c1, kc):
    src = w_ap.rearrange("(k p) n -> p k n", p=128)[:, :, c0:c1]
    cx.p.dma("pool", dst[:, :kc, :c1 - c0], src, writes=[dkey])


VEC_NAMES = ["pool_norm", "pool_scale", "ffn_norm0", "ple_norm0", "kv_norm", "attn_norm", "ffn_norm1", "ple_norm1", "final_norm"]
VI = {n: i for i, n in enumerate(VEC_NAMES)}


def common_consts(cx, es, vecs_d, cst_d):
    nc, p = cx.nc, cx.p
    cx.vecs = cx.sb(es, "vecs", [128, len(VEC_NAMES), KC], F32)
    cx.ones = cx.sb(es, "ones", [128, 128], BF16)
    cx.epsb = cx.sb(es, "epsb", [128, 1], F32)
    cx.rsw = cx.sb(es, "rsw", [32, 32], BF16)
    p.dma("sp", cx.vecs[:, :, :], vecs_d, writes=["vecs"])
    p.dma("pool", cx.rsw[:, :], cst_d, writes=["rsw"])
    p.add("dve", lambda e: e.memset(cx.ones[:, :], 1.0), writes=["ones"])
    p.add("dve", lambda e: e.memset(cx.epsb[:, :], EPS), writes=["ones"])


def ffn_alloc(cx, es, tag):
    SL = 4
    return dict(
        w1s=[cx.sb(es, tag + "w1s%d" % i, [128, KC, SL * 128], BF16) for i in range(2)],
        w3s=[cx.sb(es, tag + "w3s%d" % i, [128, KC, SL * 128], BF16) for i in range(2)],
        w2s=[cx.sb(es, tag + "w2s%d" % i, [128, SL, D], BF16) for i in range(2)],
        sl_t=[cx.sb(es, tag + "silu%d" % i, [128, G], F32) for i in range(2)],
        gbuf=[cx.sb(es, tag + "g%d" % i, [128, SL, G], BF16) for i in range(2)])


def ffn_prefetch(cx, bufs, w1_d, w3_d, w2_d, nff, tag):
    SL = 4
    nf = min(SL, nff // 128)
    ws = cx.rot(tag + "w", 2)
    bufs["pre"] = ws
    load_w(cx, bufs["w1s"][ws], (tag + "w1s", ws), w1_d, 0, nf * 128, KC)
    load_w(cx, bufs["w3s"][ws], (tag + "w3s", ws), w3_d, 0, nf * 128, KC)
    cx.p.dma("pool", bufs["w2s"][ws][:, :nf, :], w2_d[0:nf * 128, :].rearrange("(k p) n -> p k n", p=128), writes=[(tag + "w2s", ws)])


def ffn_stage(cx, bufs, h, hn, w1_d, w3_d, w2_d, nff, ngrp, tag, gate=None):
    p = cx.p
    nfc = nff // 128
    SL = 4
    w1s, w3s, w2s, sl_t, gbuf = bufs["w1s"], bufs["w3s"], bufs["w2s"], bufs["sl_t"], bufs["gbuf"]
    nsl = (nfc + SL - 1) // SL
    units = [(s_, gl) for s_ in range(nsl) for gl in range(ngrp)]
    wslot = {}

    def load(s_):
        f0 = s_ * SL
        nf = min(SL, nfc - f0)
        if s_ == 0 and bufs.get("pre") is not None:
            wslot[0] = bufs.pop("pre")
            return
        ws = cx.rot(tag + "w", 2)
        wslot[s_] = ws
        load_w(cx, w1s[ws], (tag + "w1s", ws), w1_d, f0 * 128, (f0 + nf) * 128, KC)
        load_w(cx, w3s[ws], (tag + "w3s", ws), w3_d, f0 * 128, (f0 + nf) * 128, KC)
        src2 = w2_d[f0 * 128:(f0 + nf) * 128, :].rearrange("(k p) n -> p k n", p=128)
        p.dma("pool", w2s[ws][:, :nf, :], src2, writes=[(tag + "w2s", ws)])

    def emitH(k):
        s_, gl = units[k]
        if gl == 0:
            load(s_)
        ws = wslot[s_]
        nf = min(SL, nfc - s_ * SL)
        t0 = gl * G
        gs = k % 2
        for f in range(nf):
            b1 = cx.rot(tag + "b1", 2)
            b3 = 2 + cx.rot(tag + "b3", 2)
            ss = cx.rot(tag + "sl", 2)
            for c in range(KC):
                p.add("pe", lambda e: e.matmul(cx.pb[b1][:, :], w1s[ws][:, c, f * 128:(f + 1) * 128], hn[:, c, t0:t0 + G],
                                               start=(c == 0), stop=(c == KC - 1)),
                      reads=[(tag + "w1s", ws), ("hn", gl, c)], writes=[("pb", b1)])
            for c in range(KC):
                p.add("pe", lambda e: e.matmul(cx.pb[b3][:, :], w3s[ws][:, c, f * 128:(f + 1) * 128], hn[:, c, t0:t0 + G],
                                               start=(c == 0), stop=(c == KC - 1)),
                      reads=[(tag + "w3s", ws), ("hn", gl, c)], writes=[("pb", b3)])
            p.add("act", lambda e: e.activation(out=sl_t[ss][:, :], in_=cx.pb[b1][:, :], func=AF.Silu),
                  reads=[("pb", b1)], writes=[(tag + "sl", ss)])
            if gate is None:
                p.add("dve", lambda e: e.tensor_tensor(out=gbuf[gs][:, f, :], in0=sl_t[ss][:, :], in1=cx.pb[b3][:, :], op=ALU.mult),
                      reads=[("pb", b3), (tag + "sl", ss)], writes=[(tag + "g", gs, f)])
            else:
                p.add("dve", lambda e: e.tensor_tensor(out=sl_t[ss][:, :], in0=sl_t[ss][:, :], in1=cx.pb[b3][:, :], op=ALU.mult),
                      reads=[("pb", b3), (tag + "sl", ss)], writes=[(tag + "sl", ss)])
                p.add("pool", lambda e: e.tensor_tensor(out=gbuf[gs][:, f, :], in0=sl_t[ss][:, :], in1=gate[0][:, gl, :], op=ALU.mult),
                      reads=[(tag + "sl", ss), gate[1] + (gl,)], writes=[(tag + "g", gs, f)])

    def emitY(k):
        s_, gl = units[k]
        ws = wslot[s_]
        nf = min(SL, nfc - s_ * SL)
        t0 = gl * G
        gs = k % 2
        for oc in range(KC):
            by = (4, 5, 7)[cx.rot(tag + "by", 3)]
            for f in range(nf):
                p.add("pe", lambda e: e.matmul(cx.pb[by][:, :], w2s[ws][:, f, oc * 128:(oc + 1) * 128], gbuf[gs][:, f, :],
                                               start=(f == 0), stop=(f == nf - 1)),
                      reads=[(tag + "w2s", ws), (tag + "g", gs, f)], writes=[("pb", by)])
            p.add("dve", lambda e: e.tensor_tensor(out=h[:, oc, t0:t0 + G], in0=h[:, oc, t0:t0 + G], in1=cx.pb[by][:, :], op=ALU.add),
                  reads=[("pb", by), ("h", gl, oc)], writes=[("h", gl, oc)])

    emitH(0)
    for k in range(len(units)):
        if k + 1 < len(units):
            emitH(k + 1)
        emitY(k)


def ple_alloc(cx, es, ngrp, tag):
    return dict(wg=cx.sb(es, tag + "wg", [128, KC, D], BF16), wp=cx.sb(es, tag + "wp", [128, 2, D], BF16),
                pt=cx.sb(es, tag + "pt", [128, 2, ngrp * G], BF16), sg=[cx.sb(es, tag + "sg%d" % i, [128, G], F32) for i in range(2)])


def ple_load(cx, b, pT_d, tok0, gate_d, proj_d, ngrp, tag):
    load_w(cx, b["wg"], (tag + "wg",), gate_d, 0, D, KC)
    load_w(cx, b["wp"], (tag + "wp",), proj_d, 0, D, 2)
    cx.p.dma("pool", b["pt"][:, :, :], pT_d[:, :, tok0:tok0 + ngrp * G], writes=[(tag + "pt",)])


def ple_stage(cx, b, h, hn, ngrp, tag):
    p = cx.p
    wg, wp, pt, sg = b["wg"], b["wp"], b["pt"], b["sg"]
    for gl in range(ngrp):
        t0 = gl * G
        for oc in range(KC):
            b1 = (0, 1, 4)[cx.rot(tag + "b1", 3)]
            b3 = (2, 3, 5)[cx.rot(tag + "b3", 3)]
            ss = cx.rot(tag + "sg", 2)
            for c in range(KC):
                p.add("pe", lambda e, c=c, oc=oc, b1=b1: e.matmul(cx.pb[b1][:, :], wg[:, c, oc * 128:(oc + 1) * 128], hn[:, c, t0:t0 + G],
                                                                 start=(c == 0), stop=(c == KC - 1)),
                      reads=[(tag + "wg",), ("hn", gl, c)], writes=[("pb", b1)])
            for c in range(2):
                p.add("pe", lambda e, c=c, oc=oc, b3=b3: e.matmul(cx.pb[b3][:, :], wp[:, c, oc * 128:(oc + 1) * 128], pt[:, c, t0:t0 + G],
                                                                 start=(c == 0), stop=(c == 1)),
                      reads=[(tag + "wp",), (tag + "pt",)], writes=[("pb", b3)])
            p.add("act", lambda e, b1=b1, ss=ss: e.activation(out=sg[ss][:, :], in_=cx.pb[b1][:, :], func=AF.Sigmoid),
                  reads=[("pb", b1)], writes=[(tag + "sg", ss)])
            p.add("dve", lambda e, b3=b3, ss=ss: e.tensor_tensor(out=sg[ss][:, :], in0=sg[ss][:, :], in1=cx.pb[b3][:, :], op=ALU.mult),
                  reads=[("pb", b3), (tag + "sg", ss)], writes=[(tag + "sg", ss)])
            p.add("dve", lambda e, oc=oc, ss=ss: e.tensor_tensor(out=h[:, oc, t0:t0 + G], in0=h[:, oc, t0:t0 + G], in1=sg[ss][:, :], op=ALU.add),
                  reads=[(tag + "sg", ss), ("h", gl, oc)], writes=[("h", gl, oc)])


def rmsnorm_view(cx, h, hn, gl, t0, gain, sq, rstd, tag, dst_local=False, dkey=None):
    p = cx.p
    ps = cx.pb[7]
    hkeys = [("h", gl, c) for c in range(KC)]
    for c in range(KC):
        p.add("act", lambda e, c=c: e.activation(out=sq[:, c, :G], in_=h[:, c, t0:t0 + G], func=AF.Square),
              reads=[hkeys[c]], writes=[(tag + "sq", c)])
    for c in range(KC):
        p.add("pe", lambda e, c=c: e.matmul(ps[:, :], cx.ones[:, :], sq[:, c, :G], start=(c == 0), stop=(c == KC - 1)),
              reads=[(tag + "sq", c), "ones"], writes=[("pb", 7)])
    p.add("act", lambda e: e.activation(out=rstd[:, :G], in_=ps[:, :], func=AF.Sqrt, bias=cx.epsb[:, 0:1], scale=1.0 / D),
          reads=[("pb", 7), "ones"], writes=[tag + "rstd"])
    p.add("dve", lambda e: e.reciprocal(out=rstd[:, :G], in_=rstd[:, :G]), reads=[tag + "rstd"], writes=[tag + "rstd"])
    for c in range(KC):
        d0 = 0 if dst_local else t0
        p.add("dve", lambda e, c=c: e.scalar_tensor_tensor(out=hn[:, c, d0:d0 + G], in0=h[:, c, t0:t0 + G], scalar=gain[:, c:c + 1],
                                                          in1=rstd[:, :G], op0=ALU.mult, op1=ALU.mult),
              reads=[hkeys[c], tag + "rstd", "vecs"], writes=[dkey if dkey is not None else ("hn", gl, c)])


POOL_W = (2, 4, 8, 16)


def emit_l1(cx, A, npass=NPASS):
    dbg = False
    p = cx.p
    xT, pT, cntfix_d, ropeC_d, ropeS_d, pool_w_d = A["xT"], A["pT0"], A["cntfix"], A["ropeC"], A["ropeS"], A["pool_w"]
    w1_d, w3_d, w2_d, pg_d, pp_d = A["ffn_w1"], A["ffn_w3"], A["ffn_w2"], A["ple_gate0"], A["ple_proj0"]
    wk_d, wv_d, wq_d = A["w_k"], A["w_v"], A["w_q"]
    hT_o, KT_s, QT_o, V_s, km_o = A["hscr"], A["KTscr"], A["QTscr"], A["Vscr"], A["kmscr"]
    with ExitStack() as es0:
        h = cx.sb(es0, "h", [128, KC, TP], F32)
        hn = cx.sb(es0, "hn", [128, KC, TP], BF16)
        sq = cx.sb(es0, "sq", [128, KC, G + HALO], BF16)
        rstd = cx.sb(es0, "rstd", [128, G + HALO], F32)
        kmT = cx.sb(es0, "kmT", [128, KC, 2 * NGRP], F32)
        vec = lambda n: cx.vecs[:, VI[n], :]
        for pp in range(npass):
            with ExitStack() as es:
                xhs = [cx.sb(es, "xh%d" % i, [128, KC, G + HALO], F32) for i in range(2)]
                xn = cx.sb(es, "xn", [128, KC, G + HALO], F32)
                st = [cx.sb(es, "st%d" % i, [128, G + HALO], F32) for i in range(4)]
                stp = [cx.sb(es, "stp%d" % i, [128, G + HALO], F32) for i in range(4)]
                pooled = cx.sb(es, "pooled", [128, KC, G], BF16)
                pw = cx.sb(es, "pw", [128, 4, 2, 256], BF16)
                cfix = cx.sb(es, "cfix", [128, KC, HALO], F32)
                tmpf = cx.sb(es, "tmpf", [128, HALO], F32)
                NCOL = G + HALO
                for g4 in range(4):
                    p.dma("pool", pw[:, g4, :, :], pool_w_d[g4].rearrange("(k p) n -> p k n", p=128), writes=[("pw", g4)])
                p.dma("sp", cfix[:, :, :], cntfix_d, writes=["cfix"])
                for gl in range(GP):
                    g = pp * GP + gl
                    t0 = gl * G
                    xh = xhs[gl % 2]
                    xk = ("xh", gl % 2)
                    p.dma("sp", xh[:, :, :], xT[:, :, g, :], writes=[xk])
                    for c in range(KC):
                        p.add("act", lambda e, c=c: e.activation(out=sq[:, c, :], in_=xh[:, c, :], func=AF.Square),
                              reads=[xk], writes=[("psq", c)])
                    for (b, lo, hi) in ((6, 0, HALO), (7, HALO, NCOL)):
                        for c in range(KC):
                            p.add("pe", lambda e, c=c, b=b, lo=lo, hi=hi: e.matmul(cx.pb[b][:, :hi - lo], cx.ones[:, :], sq[:, c, lo:hi],
                                                                                  start=(c == 0), stop=(c == KC - 1)),
                                  reads=[("psq", c), "ones"], writes=[("pb", b)])
                        p.add("act", lambda e, b=b, lo=lo, hi=hi: e.activation(out=rstd[:, lo:hi], in_=cx.pb[b][:, :hi - lo], func=AF.Sqrt,
                                                                              bias=cx.epsb[:, 0:1], scale=1.0 / D),
                              reads=[("pb", b), "ones"], writes=["prstd"])
                    p.add("dve", lambda e: e.reciprocal(out=rstd[:, :], in_=rstd[:, :]), reads=["prstd"], writes=["prstd"])
                    gain = vec("pool_norm")
                    for c in range(KC):
                        p.add("dve", lambda e, c=c: e.scalar_tensor_tensor(out=xn[:, c, :], in0=xh[:, c, :], scalar=gain[:, c:c + 1],
                                                                          in1=rstd[:, :], op0=ALU.mult, op1=ALU.mult),
                              reads=[xk, "prstd", "vecs"], writes=[("xn", c)])
                    for c in range(KC):
                        w = POOL_W[c // 2]
                        cur = xn[:, c, :]
                        curk = ("xn", c)
                        sh = 1
                        lo = 0
                        i = 0
                        en = "pool" if c in (4, 6) else "dve"
                        stt = stp if en == "pool" else st
                        while sh < w:
                            lo2 = lo + sh
                            p.add(en, lambda e, cur=cur, lo2=lo2, sh=sh, i=i: e.tensor_tensor(out=stt[i][:, lo2:NCOL], in0=cur[:, lo2:NCOL],
                                                                                             in1=cur[:, lo2 - sh:NCOL - sh], op=ALU.add),
                                  reads=[curk], writes=[("st", en, i)])
                            cur = stt[i][:, :]
                            curk = ("st", en, i)
                            lo = lo2
                            sh *= 2
                            i += 1
                        p.add("dve", lambda e, c=c, cur=cur, w=w: e.scalar_tensor_tensor(out=pooled[:, c, :], in0=cur[:, HALO:NCOL], scalar=1.0 / w,
                                                                                        in1=xn[:, c, HALO:NCOL], op0=ALU.mult, op1=ALU.subtract),
                              reads=[curk, ("xn", c)], writes=[("pooled", c)])
                        if g == 0:
                            p.add("dve", lambda e, c=c, cur=cur: e.tensor_tensor(out=tmpf[:, :], in0=cur[:, HALO:2 * HALO], in1=cfix[:, c, :], op=ALU.mult),
                                  reads=[curk, "cfix"], writes=["tmpf"])
                            p.add("dve", lambda e, c=c: e.tensor_tensor(out=pooled[:, c, 0:HALO], in0=tmpf[:, :], in1=xn[:, c, HALO:2 * HALO], op=ALU.subtract),
                                  reads=["tmpf", ("xn", c)], writes=[("pooled", c)])
                    sc = vec("pool_scale")
                    for oc in range(KC):
                        g4 = oc // 2
                        bk = cx.rot("poolb", 2)
                        for k in range(2):
                            p.add("pe", lambda e, oc=oc, g4=g4, k=k, bk=bk: e.matmul(cx.pb[bk][:, :], pw[:, g4, k, (oc % 2) * 128:(oc % 2 + 1) * 128],
                                                                                    pooled[:, 2 * g4 + k, :], start=(k == 0), stop=(k == 1)),
                                  reads=[("pw", g4), ("pooled", 2 * g4 + k)], writes=[("pb", bk)])
                        p.add("dve", lambda e, oc=oc, bk=bk: e.scalar_tensor_tensor(out=h[:, oc, t0:t0 + G], in0=cx.pb[bk][:, :], scalar=sc[:, oc:oc + 1],
                                                                                   in1=xh[:, oc, HALO:NCOL], op0=ALU.mult, op1=ALU.add),
                              reads=[("pb", bk), xk, "vecs"], writes=[("h", gl, oc)])
                if dbg and pp == 0:
                    p.dma("sp", d_h1, h[:, :, :], reads=[("h", gl, c) for gl in range(GP) for c in range(KC)], semname="d_dbg")
                    p.dma("sp", d_xn, xn[:, :, :], reads=[("xn", c) for c in range(KC)], semname="d_dbg2")
                    p.dma("sp", d_pooled, pooled[:, :, :], reads=[("pooled", c) for c in range(KC)], semname="d_dbg3")
                p.flush()
            es_ple = ExitStack()
            pleb = ple_alloc(cx, es_ple, GP, "e")
            with ExitStack() as es:
                for gl in range(GP):
                    rmsnorm_view(cx, h, hn, gl, gl * G, vec("ffn_norm0"), sq, rstd, "n")
                ffn_stage(cx, ffn_alloc(cx, es, "f"), h, hn, w1_d, w3_d, w2_d, D_FF, GP, "f")
                ple_load(cx, pleb, pT, pp * TP, pg_d, pp_d, GP, "e")
                p.flush()
            es_kv = ExitStack()
            wa = cx.sb(es_kv, "wa", [128, KC, D], BF16)
            wb = cx.sb(es_kv, "wb", [128, KC, D], BF16)
            with ExitStack() as es:
                for gl in range(GP):
                    rmsnorm_view(cx, h, hn, gl, gl * G, vec("ple_norm0"), sq, rstd, "n")
                ple_stage(cx, pleb, h, hn, GP, "e")
                load_w(cx, wa, ("wa",), wk_d, 0, D, KC)
                load_w(cx, wb, ("wb",), wv_d, 0, D, KC)
                p.flush()
            with ExitStack() as es:
                p.dma("sp", hT_o[:, :, pp * TP:(pp + 1) * TP], h[:, :, :], reads=[("h", gl, c) for gl in range(GP) for c in range(KC)],
                      semname="d_hout")
                rc = cx.sb(es, "rc", [32, TP], F32)
                rs = cx.sb(es, "rs", [32, TP], F32)
                kraw = [cx.sb(es, "kraw%d" % i, [128, G], BF16) for i in range(3)]
                vsb = [cx.sb(es, "vsb%d" % i, [128, D], BF16) for i in range(2)]
                t1 = cx.sb(es, "t1", [32, G], F32)
                t2 = cx.sb(es, "t2", [32, G], F32)
                p.dma("sp", rc[:, :], ropeC_d[:, pp * TP:(pp + 1) * TP], writes=["rc"])
                p.dma("sp", rs[:, :], ropeS_d[:, pp * TP:(pp + 1) * TP], writes=["rs"])
                for which in ("kv", "q"):
                    if which == "kv":
                        gname = "kv_norm"
                    else:
                        load_w(cx, wa, ("wa",), wq_d, 0, D, KC)
                        gname = "attn_norm"
                    for gl in range(GP):
                        rmsnorm_view(cx, h, hn, gl, gl * G, vec(gname), sq, rstd, "n")
                    kunits = [(gl, hd) for gl in range(GP) for hd in range(KC)]

                    def emitA(k):
                        gl, hd = kunits[k]
                        t0 = gl * G
                        bk = (0, 1, 5)[k % 3]
                        ks = k % 3
                        for c in range(KC):
                            p.add("pe", lambda e: e.matmul(cx.pb[bk][:, :], wa[:, c, hd * 128:(hd + 1) * 128], hn[:, c, t0:t0 + G],
                                                           start=(c == 0), stop=(c == KC - 1)),
                                  reads=[("wa",), ("hn", gl, c)], writes=[("pb", bk)])
                        p.add("act", lambda e: e.activation(out=kraw[ks][:, :], in_=cx.pb[bk][:, :], func=AF.Copy),
                              reads=[("pb", bk)], writes=[("kraw", ks)])

                    def emitB(k):
                        gl, hd = kunits[k]
                        g = pp * GP + gl
                        t0 = gl * G
                        ks = k % 3
                        p.add("pe", lambda e: e.matmul(cx.pb[2][0:32, :], cx.rsw[:, :], kraw[ks][0:32, :], start=True, stop=True),
                              reads=[("kraw", ks), "rsw"], writes=[("pb", 2)])
                        p.add("dve", lambda e: e.tensor_tensor(out=t1[:, :], in0=kraw[ks][0:32, :], in1=rc[:, t0:t0 + G], op=ALU.mult),
                              reads=[("kraw", ks), "rc"], writes=["t1"])
                        p.add("dve", lambda e: e.tensor_tensor(out=t2[:, :], in0=cx.pb[2][0:32, :], in1=rs[:, t0:t0 + G], op=ALU.mult),
                              reads=[("pb", 2), "rs"], writes=["t2"])
                        p.add("dve", lambda e: e.tensor_tensor(out=kraw[ks][0:32, :], in0=t1[:, :], in1=t2[:, :], op=ALU.add),
                              reads=["t1", "t2"], writes=[("kraw", ks)])
                        if which == "kv":
                            p.add("dve", lambda e: e.reduce_sum(out=kmT[:, hd, 2 * g:2 * g + 2],
                                                                in_=kraw[ks][:, :].rearrange("p (a b) -> p a b", b=256), axis=AX.X),
                                  reads=[("kraw", ks)], writes=["kmT"])
                        kdst = KT_s[hd][:, g * G:(g + 1) * G] if which == "kv" else QT_o[:, hd, g * G:(g + 1) * G]
                        p.dma("sp", kdst, kraw[ks][:, :], reads=[("kraw", ks)], semname="d_kout%d" % ks)

                    emitA(0)
                    for k in range(len(kunits)):
                        if k + 1 < len(kunits):
                            emitA(k + 1)
                        emitB(k)
                    for gl in range(GP):
                        g = pp * GP + gl
                        t0 = gl * G
                        if which == "kv":
                            for tcn in range(G // 128):
                                vs = cx.rot("vsb", 2)
                                for half in range(2):
                                    bk = 3 + cx.rot("vb", 2)
                                    for c in range(KC):
                                        p.add("pe", lambda e, c=c, half=half, bk=bk, tcn=tcn: e.matmul(
                                            cx.pb[bk][:, :], hn[:, c, t0 + tcn * 128:t0 + (tcn + 1) * 128], wb[:, c, half * 512:(half + 1) * 512],
                                            start=(c == 0), stop=(c == KC - 1)),
                                            reads=[("wb",), ("hn", gl, c)], writes=[("pb", bk)])
                                    p.add("act", lambda e, bk=bk, vs=vs, half=half: e.activation(out=vsb[vs][:, half * 512:(half + 1) * 512], in_=cx.pb[bk][:, :], func=AF.Copy),
                                          reads=[("pb", bk)], writes=[("vsb", vs, half)])
                                ktg = g * 4 + tcn
                                for hh in range(KC):
                                    p.dma("sp", V_s[hh][:, ktg * 128:(ktg + 1) * 128], vsb[vs][:, hh * 128:(hh + 1) * 128],
                                          reads=[("vsb", vs, hh // 4)], semname="d_vout%d_%d" % (vs, hh // 4))
                p.flush()
            es_kv.close()
            es_ple.close()
        p.dma("sp", km_o.rearrange("p (h b) -> p h b", h=KC), kmT[:, :, :], reads=["kmT"], semname="d_kmout")
        p.flush()


BIG = 30000.0
ATT_SCALE = 128.0 ** -0.5


def attention_phase(cx, QT_d, Kg, Vg, kmg, esel_d, ident_d, gb_d, valid_d, own_d, caus_d, OT_d, cc_keys):
    p = cx.p
    with ExitStack() as es:
        kmT = cx.sb(es, "kmT", [128, KC, NB], BF16)
        ident = cx.sb(es, "ident", [128, 128], BF16)
        esel = cx.sb(es, "esel", [128, NB * 128], BF16)
        gb = cx.sb(es, "gb", [128, 32, NB], BF16)
        valid = cx.sb(es, "valid", [128, 32, NB], BF16)
        own = cx.sb(es, "own", [128, 32, NB], BF16)
        caus = cx.sb(es, "caus", [128, 4, 4, G], BF16)
        qh = [cx.sb(es, "qh%d" % i, [128, TC], BF16) for i in range(2)]
        kh = cx.sb(es, "kh", [128, S], BF16)
        vh = cx.sb(es, "vh", [128, S // 128, 128], BF16)
        mbT = [cx.sb(es, "mbT%d" % i, [128, TC], BF16) for i in range(2)]
        oh = [cx.sb(es, "oh%d" % i, [128, TC], BF16) for i in range(2)]
        pT = [cx.sb(es, "pT%d" % i, [128, G], BF16) for i in range(8)]
        gm = [cx.sb(es, "gm%d" % i, [128, NB], F32) for i in range(2)]
        m8 = [cx.sb(es, "m8%d" % i, [128, 8], F32) for i in range(2)]
        mbq = [cx.sb(es, "mbq%d" % i, [128, NB], BF16) for i in range(2)]
        rden = cx.sb(es, "rden", [128, G], F32)
        for r4 in range(4):
            p.dma("pool", kmT[:, :, r4 * 16:(r4 + 1) * 16], kmg[r4 * 128:(r4 + 1) * 128, :].rearrange("p (h b) -> p h b", h=KC),
                  reads=[cc_keys["km"]], writes=[("kmT", r4)], semname="d_kmT")
        p.dma("pool", ident[:, :], ident_d, writes=["ident"])
        p.dma("pool", esel[:, :], esel_d, writes=["esel"])
        for i2_ in range(2):
            p.add("dve", lambda e: e.memset(mbT[i2_][64:128, :], 0.0), writes=[("mbTz", i2_)])
        p.dma("pool", gb[:, :, :], gb_d, writes=["gb"])
        p.dma("pool", valid[:, :, :], valid_d, writes=["valid"])
        p.dma("pool", own[:, :, :], own_d, writes=["own"])
        p.dma("pool", caus[:, :, :, :], caus_d, writes=["caus"])
        NCH = 4
        accs = {"pool": [cx.sb(es, "accp%d" % i, [128, G], F32) for i in range(2)],
                "dve": [cx.sb(es, "accd%d" % i, [128, G], F32) for i in range(2)]}
        ones32 = cx.sb(es, "ones32", [128, 128], F32)
        p.add("dve", lambda e: e.memset(ones32[:, :], 1.0), writes=["ones32"])

        def gating_steps(hd):
            hs = hd % 2
            steps = []

            def mkA(qc):
                def step():
                    gs = qc % 2
                    p.add("pe", lambda e: e.matmul(cx.pb[6][:, :NB], qh[hs][:, qc * 128:(qc + 1) * 128], kmT[:, hd, :], start=True, stop=True),
                          reads=[("qh", hs)] + [("kmT", r4) for r4 in range(4)], writes=[("pb", 6)])
                    p.add("dve", lambda e: e.tensor_tensor(out=gm[gs][:, :], in0=cx.pb[6][:, :NB], in1=gb[:, qc, :], op=ALU.add),
                          reads=[("pb", 6), "gb"], writes=[("gm", gs)])
                    p.add("dve", lambda e: e.max(out=m8[gs][:, :], in_=gm[gs][:, :]), reads=[("gm", gs)], writes=[("m8", gs)])
                    p.add("dve", lambda e: e.tensor_scalar(gm[gs][:, :], gm[gs][:, :], m8[gs][:, 2:3], None, ALU.is_ge),
                          reads=[("gm", gs), ("m8", gs)], writes=[("gm", gs)])
                    p.add("dve", lambda e: e.tensor_tensor(out=gm[gs][:, :], in0=gm[gs][:, :], in1=valid[:, qc, :], op=ALU.mult),
                          reads=[("gm", gs), "valid"], writes=[("gm", gs)])
                    p.add("dve", lambda e: e.tensor_tensor(out=gm[gs][:, :], in0=gm[gs][:, :], in1=own[:, qc, :], op=ALU.add),
                          reads=[("gm", gs), "own"], writes=[("gm", gs)])
                    p.add("dve", lambda e: e.tensor_scalar(mbq[gs][:, :], gm[gs][:, :], 1.0, BIG, ALU.subtract, ALU.mult),
                          reads=[("gm", gs)], writes=[("mbq", gs)])
                return step

            def mkB(qc):
                def step():
                    gs = qc % 2
                    p.add("pe", lambda e: e.matmul(cx.pb[7][0:NB, 0:128], mbq[gs][:, :], ident[:, :], start=True, stop=True),
                          reads=[("mbq", gs), "ident"], writes=[("pb", 7)])
                    p.add("act", lambda e: e.activation(out=mbT[hs][0:NB, qc * 128:(qc + 1) * 128], in_=cx.pb[7][0:NB, 0:128], func=AF.Copy),
                          reads=[("pb", 7)], writes=[("mbT", hs, qc // 4)])
                return step
            nq = TC // 128
            for qc in range(nq + 1):
                if qc < nq:
                    steps.append(mkA(qc))
                if qc >= 1:
                    steps.append(mkB(qc - 1))
            return steps

        p.dma("sp", qh[0][:, :], QT_d[:, 0, :], writes=[("qh", 0)])
        for st_ in gating_steps(0):
            st_()
        def load_kv(hd):
            for c in range(NCH):
                w = S // NCH
                p.dma("sp", kh[:, c * w:(c + 1) * w], Kg[hd][c * 128:(c + 1) * 128, :], reads=[cc_keys["k", hd]], writes=[("kh", c)])
                wt = (S // 128) // NCH
                p.dma("sp", vh[:, c * wt:(c + 1) * wt, :], Vg[hd][c * 128:(c + 1) * 128, :].rearrange("p (k d) -> p k d", d=128),
                      reads=[cc_keys["v", hd]], writes=[("vh", c)])

        load_kv(0)
        for hd in range(KC):
            hs = hd % 2
            nxt = []
            if hd + 1 < KC:
                p.dma("sp", qh[1 - hs][:, :], QT_d[:, hd + 1, :], writes=[("qh", 1 - hs)])
                nxt = gating_steps(hd + 1)
            for i in range(NGRP):
                tiles = [(r4, i2, t4) for r4 in range(4) for i2 in range(i + 1) for t4 in range(4)]
                ntile = len(tiles)
                q0 = i * G
                LOOK = 3
                slots = {}
                asl = i % 2
                ob_ = 4 + (i % 2)
                for step in range(ntile + LOOK):
                    if step < ntile:
                        r4, i2, t4 = tiles[step]
                        kt = r4 * 32 + i2 * 4 + t4
                        sb_ = cx.rot("S", 4)
                        ps_ = cx.rot("pT", 8)
                        slots[step] = (ps_, kt)
                        n = r4 * 16 + i2 * 2 + t4 // 2
                        own_pair = (i2 == i)
                        kc_ = r4
                        p.add("pe", lambda e: e.matmul(cx.pb[sb_][:, :], kh[:, kt * 128:(kt + 1) * 128], qh[hs][:, q0:q0 + G], start=True, stop=False),
                              reads=[("kh", kc_), ("qh", hs)], writes=[("pb", sb_)])
                        p.add("pe", lambda e: e.matmul(cx.pb[sb_][:, :], esel[:, n * 128:(n + 1) * 128], mbT[hs][:, q0:q0 + G], start=False, stop=not own_pair),
                              reads=["esel", ("mbT", hs, i), ("mbTz", hs)], writes=[("pb", sb_)])
                        if own_pair:
                            rr, kr = r4, t4
                            p.add("pe", lambda e: e.matmul(cx.pb[sb_][:, :], ident[:, :], caus[:, rr, kr, :], start=False, stop=True),
                                  reads=["ident", "caus"], writes=[("pb", sb_)])
                        p.add("act", lambda e: e.activation(out=pT[ps_][:, :], in_=cx.pb[sb_][:, :], func=AF.Exp, scale=ATT_SCALE),
                              reads=[("pb", sb_)], writes=[("pT", ps_)])
                        if nxt and i >= NGRP - 4 and step % 6 == 1:
                            nxt.pop(0)()
                    kv = step - LOOK
                    if kv >= 0:
                        ps_, ktv = slots.pop(kv)
                        vc_ = ktv // 32
                        p.add("pe", lambda e: e.matmul(cx.pb[ob_][:, :], vh[:, ktv, :], pT[ps_][:, :], start=(kv == 0), stop=(kv == ntile - 1)),
                              reads=[("vh", vc_), ("pT", ps_)], writes=[("pb", ob_)])
                        en = "pool" if kv % 2 == 0 else "dve"
                        acc = accs[en][asl]
                        if kv < 2:
                            p.add(en, lambda e: e.tensor_copy(out=acc[:, :], in_=pT[ps_][:, :]), reads=[("pT", ps_)], writes=[("acc", en, asl)])
                        else:
                            p.add(en, lambda e: e.tensor_tensor(out=acc[:, :], in0=acc[:, :], in1=pT[ps_][:, :], op=ALU.add),
                                  reads=[("pT", ps_), ("acc", en, asl)], writes=[("acc", en, asl)])
                p.add("pe", lambda e: e.matmul(cx.pb[7][:, :], ones32[:, :], accs["pool"][asl][:, :], start=True, stop=False),
                      reads=["ones32", ("acc", "pool", asl)], writes=[("pb", 7)])
                p.add("pe", lambda e: e.matmul(cx.pb[7][:, :], ones32[:, :], accs["dve"][asl][:, :], start=False, stop=True),
                      reads=["ones32", ("acc", "dve", asl)], writes=[("pb", 7)])
                p.add("dve", lambda e: e.reciprocal(out=rden[:, :], in_=cx.pb[7][:, :]), reads=[("pb", 7)], writes=["rden"])
                p.add("dve", lambda e: e.tensor_tensor(out=oh[hs][:, q0:q0 + G], in0=cx.pb[ob_][:, :], in1=rden[:, :], op=ALU.mult),
                      reads=[("pb", ob_), "rden"], writes=[("oh", hs)])
            while nxt:
                nxt.pop(0)()
            if hd + 1 < KC:
                load_kv(hd + 1)
            p.dma("sp", OT_d[:, hd, :], oh[hs][:, :], reads=[("oh", hs)], semname="d_ohout%d" % hs)
        p.flush()


def emit_l2(cx, A, npass=NPASS, nexp=NEXP):
    dbg = False
    p = cx.p
    hT_d, pT, ident_d = A["hscr"], A["pT1"], A["ident"]
    wo_d, router_d, e1_d, e3_d, e2_d, pg_d, pp_d = A["w_o"], A["router"], A["exp_w1"], A["exp_w3"], A["exp_w2"], A["ple_gate1"], A["ple_proj1"]
    out_o, OT_d = A["outT"], A["OTscr"]
    vec = lambda n: cx.vecs[:, VI[n], :]
    if True:
        attention_phase(cx, A["QTscr"], A["Kg"], A["Vg"], A["kmg"], A["esel"], ident_d, A["gb"], A["valid"], A["own"], A["caus"], OT_d, A["cc_keys"])
        with ExitStack() as es1:
            h = cx.sb(es1, "h", [128, KC, TP], F32)
            hn = cx.sb(es1, "hn", [128, KC, TP], BF16)
            sq = cx.sb(es1, "sq", [128, KC, G], BF16)
            rstd = cx.sb(es1, "rstd", [128, G], F32)
            for pp in range(npass):
                tok0 = pp * TP
                es_moe = ExitStack()
                bufs = ffn_alloc(cx, es_moe, "m")
                with ExitStack() as es:
                    wo = cx.sb(es, "wo", [128, KC, D], BF16)
                    load_w(cx, wo, ("wo",), wo_d, 0, D, KC)
                    p.dma("sp", h[:, :, :], hT_d[:, :, tok0:tok0 + TP], writes=[("h", gl, c) for gl in range(GP) for c in range(KC)])
                    p.dma("sp", hn[:, :, :], OT_d[:, :, tok0:tok0 + TP], writes=[("hn", gl, c) for gl in range(GP) for c in range(KC)])
                    for gl in range(GP):
                        t0 = gl * G
                        for oc in range(KC):
                            bk = cx.rot("wob", 3)
                            for c in range(KC):
                                p.add("pe", lambda e: e.matmul(cx.pb[bk][:, :], wo[:, c, oc * 128:(oc + 1) * 128], hn[:, c, t0:t0 + G],
                                                               start=(c == 0), stop=(c == KC - 1)),
                                      reads=[("wo",), ("hn", gl, c)], writes=[("pb", bk)])
                            p.add("dve", lambda e: e.tensor_tensor(out=h[:, oc, t0:t0 + G], in0=h[:, oc, t0:t0 + G], in1=cx.pb[bk][:, :], op=ALU.add),
                                  reads=[("pb", bk), ("h", gl, oc)], writes=[("h", gl, oc)])
                    ffn_prefetch(cx, bufs, e1_d[0], e3_d[0], e2_d[0], E_FF, "m")
                    p.flush()
                with ExitStack() as es:
                    rt = cx.sb(es, "rt", [128, KC, NEXP], BF16)
                    ident = cx.sb(es, "identb", [128, 128], BF16)
                    lg = cx.sb(es, "lg", [128, NEXP], F32)
                    m8 = cx.sb(es, "m8r", [128, 8], F32)
                    nm1 = cx.sb(es, "nm1", [128, 1], F32)
                    ex = cx.sb(es, "ex", [128, NEXP], F32)
                    den = cx.sb(es, "den", [128, 1], F32)
                    gates = cx.sb(es, "gates", [128, GP * 4, NEXP], F32)
                    dg = [cx.sb(es, "dg%d" % i, [128, 128], BF16) for i in range(2)]
                    gbcs = [cx.sb(es, "gbc%d" % i, [128, GP, G], BF16) for i in range(2)]
                    load_w(cx, rt, ("rt",), router_d, 0, NEXP, KC)
                    p.dma("pool", ident[:, :], ident_d, writes=["identb"])
                    for gl in range(GP):
                        rmsnorm_view(cx, h, hn, gl, gl * G, vec("ffn_norm1"), sq, rstd, "n")
                    for tcn in range(GP * 4):
                        gl = tcn // 4
                        for c in range(KC):
                            p.add("pe", lambda e: e.matmul(cx.pb[6][:, :NEXP], hn[:, c, tcn * 128:(tcn + 1) * 128], rt[:, c, :], start=(c == 0), stop=(c == KC - 1)),
                                  reads=[("rt",), ("hn", gl, c)], writes=[("pb", 6)])
                        p.add("dve", lambda e: e.tensor_copy(out=lg[:, :], in_=cx.pb[6][:, :NEXP]), reads=[("pb", 6)], writes=["lg"])
                        p.add("dve", lambda e: e.max(out=m8[:, :], in_=lg[:, :]), reads=["lg"], writes=["m8r"])
                        p.add("dve", lambda e: e.tensor_scalar(nm1[:, :], m8[:, 0:1], -1.0, None, ALU.mult), reads=["m8r"], writes=["nm1"])
                        p.add("act", lambda e: e.activation(out=ex[:, :], in_=lg[:, :], func=AF.Exp, bias=nm1[:, 0:1], scale=1.0),
                              reads=["lg", "nm1"], writes=["ex"])
                        p.add("dve", lambda e: e.tensor_scalar(lg[:, :], lg[:, :], m8[:, 1:2], None, ALU.is_ge), reads=["lg", "m8r"], writes=["lg"])
                        p.add("dve", lambda e: e.tensor_tensor(out=ex[:, :], in0=ex[:, :], in1=lg[:, :], op=ALU.mult), reads=["ex", "lg"], writes=["ex"])
                        p.add("dve", lambda e: e.reduce_sum(out=den[:, :], in_=ex[:, :], axis=AX.X), reads=["ex"], writes=["den"])
                        p.add("dve", lambda e: e.reciprocal(out=den[:, :], in_=den[:, :]), reads=["den"], writes=["den"])
                        p.add("dve", lambda e: e.tensor_scalar(gates[:, tcn, :], ex[:, :], den[:, 0:1], None, ALU.mult), reads=["ex", "den"], writes=[("gates", tcn)])
                    if dbg and pp == 0:
                        p.dma("sp", d_gates, gates[:, :, :], reads=[("gates", t) for t in range(GP * 4)], semname="d_dbg5")
                    for ex_i in range(nexp):
                        gbc = gbcs[ex_i % 2]
                        gkey = ("gbc", ex_i % 2)
                        for gl in range(GP):
                            for t4 in range(4):
                                tcn = gl * 4 + t4
                                ds = cx.rot("dg", 2)
                                p.add("dve", lambda e: e.tensor_scalar(dg[ds][:, :], ident[:, :], gates[:, tcn, ex_i:ex_i + 1], None, ALU.mult),
                                      reads=["identb", ("gates", tcn)], writes=[("dg", ds)])
                                p.add("pe", lambda e: e.matmul(cx.pb[6][:, t4 * 128:(t4 + 1) * 128], cx.ones[:, :], dg[ds][:, :], start=True, stop=True),
                                      reads=["ones", ("dg", ds)], writes=[("pb", 6, t4)])
                            p.add("act", lambda e: e.activation(out=gbc[:, gl, :], in_=cx.pb[6][:, :], func=AF.Copy),
                                  reads=[("pb", 6, t4) for t4 in range(4)], writes=[gkey + (gl,)])
                        ffn_stage(cx, bufs, h, hn, e1_d[ex_i], e3_d[ex_i], e2_d[ex_i], E_FF, GP, "m", gate=(gbc, gkey))
                    p.flush()
                es_moe.close()
                with ExitStack() as es:
                    for gl in range(GP):
                        rmsnorm_view(cx, h, hn, gl, gl * G, vec("ple_norm1"), sq, rstd, "n")
                    pleb = ple_alloc(cx, es, GP, "e")
                    ple_load(cx, pleb, pT, tok0, pg_d, pp_d, GP, "e")
                    ple_stage(cx, pleb, h, hn, GP, "e")
                    p.flush()
                with ExitStack() as es:
                    ob = [cx.sb(es, "ob%d" % i, [128, KC, G], F32) for i in range(2)]
                    for gl in range(GP):
                        os_ = gl % 2
                        rmsnorm_view(cx, h, ob[os_], gl, gl * G, vec("final_norm"), sq, rstd, "n", dst_local=True, dkey=("ob", os_))
                        p.dma("sp", out_o[:, :, tok0 + gl * G:tok0 + (gl + 1) * G], ob[os_][:, :, :], reads=[("ob", os_)], semname="d_out%d" % os_)
                    p.flush()


def core_positions(r):
    return np.concatenate([np.arange(512 * (r + 4 * i), 512 * (r + 4 * i) + 512) for i in range(NGRP)])


def fm(a):
    T, nf = a.shape
    return np.ascontiguousarray(a.reshape(T, nf // 128, 128).transpose(2, 1, 0))


def vec_fm(v):
    return np.ascontiguousarray(v.reshape(KC, 128).T)


def rope_tables(pos):
    half = 16
    inv_freq = np.float32(500000.0) ** (-(np.arange(0, 32, 2, dtype=np.float32) / np.float32(32)))
    ang = pos.astype(np.float32)[None, :] * inv_freq[:, None].astype(np.float32)
    c = np.cos(ang).astype(np.float32)
    s = np.sin(ang).astype(np.float32)
    return np.concatenate([c, c], 0), np.concatenate([-s, s], 0)


def build(npass=NPASS, nexp=NEXP):
    nc = bass.Bass("TRN2", target_bir_lowering=False)

    def din(name, shape, dt=F32):
        return nc.dram_tensor(name, list(shape), dt, kind="ExternalInput").ap()

    def scr(name, shape, dt):
        return nc.dram_tensor(name, list(shape), dt)

    A = dict(
        xT=din("xT", [128, KC, NGRP, G + HALO]), pT0=din("pT0", [128, 2, TC]), pT1=din("pT1", [128, 2, TC]),
        cntfix=din("cntfix", [128, KC, HALO]), ropeC=din("ropeC", [32, TC]), ropeS=din("ropeS", [32, TC]),
        pool_w=din("pool_w", [4, 256, 256]), ffn_w1=din("ffn_w1", [D, D_FF]), ffn_w3=din("ffn_w3", [D, D_FF]), ffn_w2=din("ffn_w2", [D_FF, D]),
        ple_gate0=din("ple_gate0", [D, D]), ple_proj0=din("ple_proj0", [PLE, D]), ple_gate1=din("ple_gate1", [D, D]), ple_proj1=din("ple_proj1", [PLE, D]),
        w_k=din("w_k", [D, D]), w_v=din("w_v", [D, D]), w_q=din("w_q", [D, D]), w_o=din("w_o", [D, D]),
        router=din("router", [D, NEXP]), exp_w1=din("exp_w1", [NEXP, D, E_FF]), exp_w3=din("exp_w3", [NEXP, D, E_FF]), exp_w2=din("exp_w2", [NEXP, E_FF, D]),
        esel=din("esel", [128, NB * 128]), ident=din("ident", [128, 128]), gb=din("gb", [128, 32, NB]), valid=din("valid", [128, 32, NB]),
        own=din("own", [128, 32, NB]), caus=din("caus", [128, 4, 4, G]),
    )
    vecs_d = din("vecs", [128, len(VEC_NAMES), KC])
    rsw_d = din("rsw", [32, 32])
    A["outT"] = nc.dram_tensor("outT", [128, KC, TC], F32, kind="ExternalOutput").ap()
    A["hscr"] = scr("hscr", [128, KC, TC], F32).ap()
    A["QTscr"] = scr("QTscr", [128, KC, TC], BF16).ap()
    A["OTscr"] = scr("OTscr", [128, KC, TC], BF16).ap()
    kts = [scr("KTscr%d" % i, [128, TC], BF16) for i in range(KC)]
    vss = [scr("Vscr%d" % i, [128, TC], BF16) for i in range(KC)]
    kgs = [scr("Kg%d" % i, [4 * 128, TC], BF16) for i in range(KC)]
    vgs = [scr("Vg%d" % i, [4 * 128, TC], BF16) for i in range(KC)]
    kms = scr("kmscr", [128, KC * 16], F32)
    kmg = scr("kmg", [4 * 128, KC * 16], F32)
    A["KTscr"] = [t.ap() for t in kts]
    A["Vscr"] = [t.ap() for t in vss]
    A["Kg"] = [t.ap() for t in kgs]
    A["Vg"] = [t.ap() for t in vgs]
    A["kmscr"] = kms.ap()
    A["kmg"] = kmg.ap()
    groups = [[0, 1, 2, 3], [4, 5, 6, 7]]
    with ExitStack() as es0:
        cx = Ctx(nc, es0)
        p = cx.p
        common_consts(cx, es0, vecs_d, rsw_d)
        p.flush()
        emit_l1(cx, A, npass)
        cck = {"km": ("cc", "km")}
        p.cc(kms.ap().opt(), kmg.ap().opt(), groups, reads=[], writes=[cck["km"]], semname="cc_km")
        for hd in range(KC):
            cck["k", hd] = ("cc", "k", hd)
            cck["v", hd] = ("cc", "v", hd)
            p.cc(kts[hd].ap().opt(), kgs[hd].ap().opt(), groups, reads=[], writes=[cck["k", hd]], semname="cc_k%d" % hd)
            p.cc(vss[hd].ap().opt(), vgs[hd].ap().opt(), groups, reads=[], writes=[cck["v", hd]], semname="cc_v%d" % hd)
        A["cc_keys"] = cck
        emit_l2(cx, A, npass, nexp)
    return nc


def seq_block_of(n2):
    r4, rem = n2 // 16, n2 % 16
    return 2 * (r4 + 4 * (rem // 2)) + rem % 2


def l2_consts(r):
    gb = np.zeros((128, 32, NB), np.float32)
    valid = np.zeros((128, 32, NB), np.float32)
    own = np.zeros((128, 32, NB), np.float32)
    nseq = np.array([seq_block_of(n2) for n2 in range(NB)])
    for qc in range(32):
        i = qc // 4
        j = 2 * (r + 4 * i) + (qc % 4) // 2
        gb[:, qc, nseq >= j] = -1e30
        valid[:, qc, nseq < j] = 1.0
        own[:, qc, nseq == j] = 1.0
    caus = np.zeros((128, 4, 4, G), np.float32)
    pk = np.arange(128)[:, None]
    t = np.arange(G)[None, :]
    for kr in range(4):
        same = (kr // 2) == (t // 256)
        fut = ((kr % 2) * 128 + pk) > (t % 256)
        caus[:, r, kr, :] = np.where(same & fut, -BIG, 0.0)
    return gb, valid, own, caus


def make_inputs(inp):
    x, p = inp["x"], inp["p"]
    vecs = np.stack([vec_fm(v) for v in (inp["pool_norm"][0], inp["pool_scale"][0], inp["ffn_norm"][0], inp["ple_norm"][0], inp["kv_norm"],
                                         inp["attn_norm"][0], inp["ffn_norm"][1], inp["ple_norm"][1], inp["final_norm"])], axis=1)
    rsw = np.zeros((32, 32), np.float32)
    for i in range(16):
        rsw[16 + i, i] = 1.0
        rsw[i, 16 + i] = 1.0
    esel = np.zeros((128, NB * 128), np.float32)
    for n in range(NB):
        esel[n, n * 128:(n + 1) * 128] = 1.0
    shared = dict(vecs=np.ascontiguousarray(vecs, dtype=np.float32), rsw=rsw, pool_w=np.ascontiguousarray(inp["pool_w"][0]),
                  ffn_w1=inp["ffn_w1"][0], ffn_w3=inp["ffn_w3"][0], ffn_w2=inp["ffn_w2"][0], ple_gate0=inp["ple_gate"][0],
                  ple_proj0=inp["ple_proj"][0], ple_gate1=inp["ple_gate"][1], ple_proj1=inp["ple_proj"][1],
                  w_k=inp["w_k"], w_v=inp["w_v"], w_q=inp["w_q"][0], w_o=inp["w_o"][0], router=inp["router"][0],
                  exp_w1=inp["exp_w1"][0], exp_w3=inp["exp_w3"][0], exp_w2=inp["exp_w2"][0], esel=esel, ident=np.eye(128, dtype=np.float32))
    maps = []
    for c in range(8):
        b, r = c // 4, c % 4
        pos = core_positions(r)
        xT = np.zeros((128, KC, NGRP, G + HALO), np.float32)
        for i in range(NGRP):
            s0 = 512 * (r + 4 * i)
            lo = max(s0 - HALO, 0)
            seg = x[b, lo:s0 + G]
            xT[:, :, i, G + HALO - seg.shape[0]:] = seg.reshape(-1, KC, 128).transpose(2, 1, 0)
        cntfix = np.zeros((128, KC, HALO), np.float32)
        for cch in range(KC):
            w = POOL_W[cch // 2]
            if r == 0:
                cntfix[:, cch, :] = 1.0 / np.minimum(np.arange(HALO) + 1, w).astype(np.float32)
            else:
                cntfix[:, cch, :] = 1.0 / w
        rc, rs = rope_tables(pos)
        gb, valid, own, caus = l2_consts(r)
        m = dict(shared)
        m.update(xT=xT, pT0=fm(p[0, b][pos]), pT1=fm(p[1, b][pos]), cntfix=cntfix, ropeC=rc, ropeS=rs, gb=gb, valid=valid, own=own, caus=caus)
        maps.append(m)
    return maps


def kernel(**inputs):
    inp = {k: np.asarray(v) for k, v in inputs.items()}
    nc = build()
    maps = make_inputs(inp)
    res = run_bass_kernel_spmd(nc, maps, core_ids=list(range(8)))
    out = np.zeros((B, S, D), np.float32)
    for c in range(8):
        b, r = c // 4, c % 4
        pos = core_positions(r)
        out[b, pos] = np.asarray(res.results[c]["outT"]).transpose(2, 1, 0).reshape(TC, D)
    return out
```
